# Optimizing a Trainium2 kernel written in Bass

```python
import jax, jax.numpy as jnp
from jax import lax
import numpy as np


D_MODEL = 2048
BATCH = 1
SEQ = 16384
DEPTH = 2

HEAD_DIM = 128
N_MAIN_HEADS = 12
N_KV_HEADS = 4
GROUP = N_MAIN_HEADS // N_KV_HEADS
N_MEM_HEADS = 4
N_MEM_TOKENS = 256
IDX_HEADS = 16
IDX_DIM = 64
IDX_TOPK_MAX = 256
MOBA_BLOCK = 256
MOBA_TOPK_MAX = 3
D_FF = 5632
ROPE_THETA = 10000.0
RMS_EPS = 1e-6
Q_CHUNK_A = 128
Q_CHUNK_B = 64
N_A_LAYERS = DEPTH // 2
N_B_LAYERS = DEPTH - N_A_LAYERS

A_SIZES = (N_MAIN_HEADS * HEAD_DIM, N_KV_HEADS * HEAD_DIM, N_KV_HEADS * HEAD_DIM,
           IDX_HEADS * IDX_DIM, IDX_DIM, IDX_HEADS, N_MEM_HEADS * HEAD_DIM)
B_SIZES = (N_MAIN_HEADS * HEAD_DIM, N_MEM_HEADS * HEAD_DIM)
MIX_WIDTH = (N_MAIN_HEADS + N_MEM_HEADS) * HEAD_DIM

kernel_name = 'yoco_dsa_moba_macaron_memory'


def split_cols(t, sizes):
    idx = [int(c) for c in np.cumsum(sizes)[:-1]]
    return jnp.split(t, idx, axis=-1)


def rms_norm(x, g):
    xf = x.astype(jnp.float32)
    y = xf * lax.rsqrt(jnp.mean(xf * xf, axis=-1, keepdims=True) + RMS_EPS)
    return (y * g.astype(jnp.float32)).astype(x.dtype)


def rope_tables(positions, dim):
    inv = 1.0 / (ROPE_THETA ** (jnp.arange(0, dim, 2, dtype=jnp.float32) / dim))
    ang = positions.astype(jnp.float32)[..., None] * inv
    return jnp.cos(ang), jnp.sin(ang)


def apply_rope(t, cos, sin):
    t1, t2 = jnp.split(t.astype(jnp.float32), 2, axis=-1)
    c = cos[:, :, None, :]
    s = sin[:, :, None, :]
    return jnp.concatenate([t1 * c - t2 * s, t1 * s + t2 * c], axis=-1).astype(t.dtype)


def swiglu(h, w_gate_up, w_down):
    g, u = jnp.split(h @ w_gate_up, 2, axis=-1)
    return (jax.nn.silu(g) * u) @ w_down


def to_chunks(t, chunk):
    b, s = t.shape[:2]
    return jnp.moveaxis(t.reshape((b, s // chunk, chunk) + t.shape[2:]), 1, 0)


def dsa_attention(q, k, v, q_idx, k_idx, w_idx, topk):
    b, s = q.shape[:2]
    n_chunks = s // Q_CHUNK_A
    key_pos = jnp.arange(s)
    bidx = jnp.arange(b)[:, None, None]
    scale = HEAD_DIM ** -0.5

    def one_chunk(args):
        qc, qic, wc, c0 = args
        qpos = c0 + jnp.arange(Q_CHUNK_A)
        dots = jnp.einsum('bqhd,bsd->bqhs', qic, k_idx).astype(jnp.float32)
        iscore = jnp.einsum('bqh,bqhs->bqs', wc.astype(jnp.float32), jax.nn.relu(dots))
        causal = key_pos[None, :] <= qpos[:, None]
        iscore = jnp.where(causal[None], iscore, -jnp.inf)
        _, sel = lax.top_k(iscore, topk)
        valid = sel <= qpos[None, :, None]
        ks = k[bidx, sel]
        vs = v[bidx, sel]
        qg = qc.reshape(b, Q_CHUNK_A, N_KV_HEADS, GROUP, HEAD_DIM)
        logits = jnp.einsum('bqgrd,bqkgd->bqgrk', qg, ks).astype(jnp.float32) * scale
        logits = jnp.where(valid[:, :, None, None, :], logits, -jnp.inf)
        p = jax.nn.softmax(logits, axis=-1).astype(vs.dtype)
        o = jnp.einsum('bqgrk,bqkgd->bqgrd', p, vs)
        return o.reshape(b, Q_CHUNK_A, N_MAIN_HEADS * HEAD_DIM)

    starts = jnp.arange(n_chunks, dtype=jnp.int32) * Q_CHUNK_A
    out = lax.map(one_chunk, (to_chunks(q, Q_CHUNK_A), to_chunks(q_idx, Q_CHUNK_A),
                              to_chunks(w_idx, Q_CHUNK_A), starts))
    return jnp.moveaxis(out, 0, 1).reshape(b, s, N_MAIN_HEADS * HEAD_DIM)


def moba_attention(q, k_blocks, v_blocks, k_means, n_sel):
    b, s = q.shape[:2]
    nb = k_blocks.shape[2]
    n_chunks = s // Q_CHUNK_B
    scale = HEAD_DIM ** -0.5
    bidx = jnp.arange(b)[:, None, None, None]
    hidx = (jnp.arange(N_MAIN_HEADS) // GROUP)[None, None, :, None]
    block_ids = jnp.arange(nb)

    def one_chunk(args):
        qc, c0 = args
        qpos = c0 + jnp.arange(Q_CHUNK_B)
        cur = c0 // MOBA_BLOCK
        qg = qc.reshape(b, Q_CHUNK_B, N_KV_HEADS, GROUP, HEAD_DIM)
        gate = jnp.einsum('bqgrd,bgnd->bqgrn', qg, k_means).astype(jnp.float32)
        gate = gate.reshape(b, Q_CHUNK_B, N_MAIN_HEADS, nb)
        gate = jnp.where(block_ids < cur, gate, -jnp.inf)
        _, sel = lax.top_k(gate, n_sel)
        sel_valid = sel < cur
        ks = k_blocks[bidx, hidx, sel]
        vs = v_blocks[bidx, hidx, sel]
        lp = jnp.einsum('bqhd,bqhnkd->bqhnk', qc, ks).astype(jnp.float32) * scale
        lp = jnp.where(sel_valid[..., None], lp, -jnp.inf)
        lp = lp.reshape(b, Q_CHUNK_B, N_MAIN_HEADS, n_sel * MOBA_BLOCK)
        k_own = lax.dynamic_index_in_dim(k_blocks, cur, axis=2, keepdims=False)
        v_own = lax.dynamic_index_in_dim(v_blocks, cur, axis=2, keepdims=False)
        own_pos = cur * MOBA_BLOCK + jnp.arange(MOBA_BLOCK)
        lo = jnp.einsum('bqgrd,bgkd->bqgrk', qg, k_own).astype(jnp.float32) * scale
        lo = lo.reshape(b, Q_CHUNK_B, N_MAIN_HEADS, MOBA_BLOCK)
        lo = jnp.where((own_pos[None, :] <= qpos[:, None])[None, :, None, :], lo, -jnp.inf)
        p = jax.nn.softmax(jnp.concatenate([lp, lo], axis=-1), axis=-1).astype(vs.dtype)
        p_sel = p[..., :n_sel * MOBA_BLOCK].reshape(b, Q_CHUNK_B, N_MAIN_HEADS, n_sel, MOBA_BLOCK)
        p_own = p[..., n_sel * MOBA_BLOCK:].reshape(b, Q_CHUNK_B, N_KV_HEADS, GROUP, MOBA_BLOCK)
        o = (jnp.einsum('bqhnk,bqhnkd->bqhd', p_sel, vs)
             + jnp.einsum('bqgrk,bgkd->bqgrd', p_own, v_own).reshape(b, Q_CHUNK_B, N_MAIN_HEADS, HEAD_DIM))
        return o.reshape(b, Q_CHUNK_B, N_MAIN_HEADS * HEAD_DIM)

    starts = jnp.arange(n_chunks, dtype=jnp.int32) * Q_CHUNK_B
    out = lax.map(one_chunk, (to_chunks(q, Q_CHUNK_B), starts))
    return jnp.moveaxis(out, 0, 1).reshape(b, s, N_MAIN_HEADS * HEAD_DIM)


def memory_attention(q_mem, mem_kv):
    b, s = q_mem.shape[:2]
    m = mem_kv.shape[1]
    qm = q_mem.reshape(b, s, N_MEM_HEADS, HEAD_DIM)
    km, vm = jnp.split(mem_kv, 2, axis=-1)
    km = km.reshape(b, m, N_MEM_HEADS, HEAD_DIM)
    vm = vm.reshape(b, m, N_MEM_HEADS, HEAD_DIM)
    logits = jnp.einsum('bshd,bmhd->bhsm', qm, km).astype(jnp.float32) * (HEAD_DIM ** -0.5)
    p = jax.nn.softmax(logits, axis=-1).astype(vm.dtype)
    return jnp.einsum('bhsm,bmhd->bshd', p, vm).reshape(b, s, N_MEM_HEADS * HEAD_DIM)


def setup_inputs(seed: int = 0) -> dict:
    key = jax.random.key(seed)
    ks = jax.random.split(key, 24)

    def w(k, shape, fan_in):
        return jax.random.normal(k, shape, jnp.float32) * (fan_in ** -0.5)

    def gain(k, shape):
        return 1.0 + 0.02 * jax.random.normal(k, shape, jnp.float32)

    a_in = sum(A_SIZES)
    b_in = sum(B_SIZES)
    kv_w = 2 * N_KV_HEADS * HEAD_DIM
    mem_w = 2 * N_MEM_HEADS * HEAD_DIM
    return {
        'x': jax.random.normal(ks[0], (BATCH, SEQ, D_MODEL), jnp.float32),
        'mem': jax.random.normal(ks[1], (BATCH, N_MEM_TOKENS, D_MODEL), jnp.float32),
        'positions': jnp.broadcast_to(jnp.arange(SEQ, dtype=jnp.int32), (BATCH, SEQ)),
        'ffn1_norm': gain(ks[2], (DEPTH, D_MODEL)),
        'ffn1_w_gate_up': w(ks[3], (DEPTH, D_MODEL, 2 * D_FF), D_MODEL),
        'ffn1_w_down': w(ks[4], (DEPTH, D_FF, D_MODEL), D_FF),
        'attn_norm': gain(ks[5], (DEPTH, D_MODEL)),
        'mem_norm': gain(ks[6], (DEPTH, D_MODEL)),
        'a_w_in': w(ks[7], (N_A_LAYERS, D_MODEL, a_in), D_MODEL),
        'idx_k_norm': gain(ks[8], (N_A_LAYERS, IDX_DIM)),
        'b_w_in': w(ks[9], (N_B_LAYERS, D_MODEL, b_in), D_MODEL),
        'w_mem_kv': w(ks[10], (DEPTH, D_MODEL, mem_w), D_MODEL),
        'w_out': w(ks[11], (DEPTH, MIX_WIDTH, D_MODEL), MIX_WIDTH),
        'ffn2_norm': gain(ks[12], (DEPTH, D_MODEL)),
        'ffn2_w_gate_up': w(ks[13], (DEPTH, D_MODEL, 2 * D_FF), D_MODEL),
        'ffn2_w_down': w(ks[14], (DEPTH, D_FF, D_MODEL), D_FF),
        'kv_norm': gain(ks[15], (D_MODEL,)),
        'w_kv_shared': w(ks[16], (D_MODEL, kv_w), D_MODEL),
        'final_norm': gain(ks[17], (D_MODEL,)),
    }


def reference(x, mem, positions, ffn1_norm, ffn1_w_gate_up, ffn1_w_down, attn_norm, mem_norm,
              a_w_in, idx_k_norm, b_w_in, w_mem_kv, w_out, ffn2_norm, ffn2_w_gate_up, ffn2_w_down,
              kv_norm, w_kv_shared, final_norm):
    b, s, _ = x.shape
    cos_h, sin_h = rope_tables(positions, HEAD_DIM)
    cos_i, sin_i = rope_tables(positions, IDX_DIM)
    topk = min(IDX_TOPK_MAX, s // 4)
    nb = -(-s // MOBA_BLOCK)
    n_sel = min(MOBA_TOPK_MAX, max(nb - 1, 1))
    idx_w_scale = (IDX_HEADS ** -0.5) * (IDX_DIM ** -0.5)

    k_blocks = v_blocks = k_means = None
    for i in range(DEPTH):
        if i == N_A_LAYERS:
            hk = rms_norm(x, kv_norm)
            k_sh, v_sh = jnp.split(hk @ w_kv_shared, 2, axis=-1)
            k_sh = apply_rope(k_sh.reshape(b, s, N_KV_HEADS, HEAD_DIM), cos_h, sin_h)
            v_sh = v_sh.reshape(b, s, N_KV_HEADS, HEAD_DIM)
            pad = nb * MOBA_BLOCK - s
            k_sh = jnp.pad(k_sh, ((0, 0), (0, pad), (0, 0), (0, 0)))
            v_sh = jnp.pad(v_sh, ((0, 0), (0, pad), (0, 0), (0, 0)))
            k_blocks = jnp.transpose(k_sh.reshape(b, nb, MOBA_BLOCK, N_KV_HEADS, HEAD_DIM), (0, 3, 1, 2, 4))
            v_blocks = jnp.transpose(v_sh.reshape(b, nb, MOBA_BLOCK, N_KV_HEADS, HEAD_DIM), (0, 3, 1, 2, 4))
            k_means = jnp.mean(k_blocks.astype(jnp.float32), axis=3).astype(k_blocks.dtype)

        x = x + 0.5 * swiglu(rms_norm(x, ffn1_norm[i]), ffn1_w_gate_up[i], ffn1_w_down[i])

        h = rms_norm(x, attn_norm[i])
        mem_kv = rms_norm(mem, mem_norm[i]) @ w_mem_kv[i]
        if i < N_A_LAYERS:
            q, k, v, qi, ki, wi, qm = split_cols(h @ a_w_in[i], A_SIZES)
            q = apply_rope(q.reshape(b, s, N_MAIN_HEADS, HEAD_DIM), cos_h, sin_h)
            k = apply_rope(k.reshape(b, s, N_KV_HEADS, HEAD_DIM), cos_h, sin_h)
            v = v.reshape(b, s, N_KV_HEADS, HEAD_DIM)
            qi = apply_rope(qi.reshape(b, s, IDX_HEADS, IDX_DIM), cos_i, sin_i)
            ki = apply_rope(rms_norm(ki, idx_k_norm[i])[:, :, None, :], cos_i, sin_i)[:, :, 0, :]
            wi = wi * idx_w_scale
            o_main = dsa_attention(q, k, v, qi, ki, wi, topk)
        else:
            j = i - N_A_LAYERS
            q, qm = split_cols(h @ b_w_in[j], B_SIZES)
            q = apply_rope(q.reshape(b, s, N_MAIN_HEADS, HEAD_DIM), cos_h, sin_h)
            o_main = moba_attention(q, k_blocks, v_blocks, k_means, n_sel)
        o_mem = memory_attention(qm, mem_kv)
        x = x + jnp.concatenate([o_main, o_mem], axis=-1) @ w_out[i]

        x = x + 0.5 * swiglu(rms_norm(x, ffn2_norm[i]), ffn2_w_gate_up[i], ffn2_w_down[i])

    return rms_norm(x, final_norm)
```

```python
import contextlib
import numpy as np
import ml_dtypes
import concourse.bass as bass
import concourse.mybir as mybir
from concourse.bass_utils import run_bass_kernel_spmd
F32 = mybir.dt.float32
BF16 = mybir.dt.bfloat16
I32 = mybir.dt.int32
AF = mybir.ActivationFunctionType
ALU = mybir.AluOpType
AX = mybir.AxisListType


class Buf:
    def __init__(self, kb, name, t):
        self.kb = kb
        self.name = name
        self.t = t
        self.w = {}
        self.r = {}
        self.dsem = None
        self.dcount = 0

    def __getitem__(self, k):
        return self.t[k]

    def ap(self):
        return self.t.ap() if hasattr(self.t, "ap") else self.t[:]


class Eng:
    def __init__(self, kb, name, h):
        self.kb = kb
        self.name = name
        self.h = h
        self.sem = None
        self.count = 0
        self.known = {}


class KB:
    def __init__(self, same_engine_sync=True):
        self.nc = bass.Bass("TRN2", target_bir_lowering=False)
        self.es = contextlib.ExitStack()
        nc = self.nc
        self.same = same_engine_sync
        self.pe = Eng(self, "pe", nc.tensor)
        self.act = Eng(self, "act", nc.scalar)
        self.dve = Eng(self, "dve", nc.vector)
        self.pool = Eng(self, "pool", nc.gpsimd)
        self.sp = Eng(self, "sp", nc.sync)
        self.engs = [self.pe, self.act, self.dve, self.pool, self.sp]
        for e in self.engs:
            e.sem = self.es.enter_context(nc.semaphore("sem_" + e.name))
        self.nsem = 0
        self.out_tickets = []
        self.n_inst = 0

    def sb(self, name, shape, dtype, stack=None):
        t = (stack or self.es).enter_context(self.nc.sbuf_tensor(name, list(shape), dtype))
        return Buf(self, name, t)

    def ps(self, name, shape, dtype=F32, stack=None):
        t = (stack or self.es).enter_context(self.nc.psum_tensor(name, list(shape), dtype))
        return Buf(self, name, t)

    def dram(self, name, shape, dtype, kind="Internal"):
        t = self.nc.dram_tensor(name, list(shape), dtype, kind=kind)
        b = Buf(self, name, t.ap())
        b.kind = kind
        return b

    def view(self, buf, name=None):
        return Buf(self, name or buf.name + "_v", buf.t)

    def _need(self, eng, tickets):
        for key, (sem, val) in tickets.items():
            if not self.same and key == id(eng.sem) and eng is not self.pe:
                continue
            if eng is self.pe and key == id(eng.sem):
                continue
            if eng.known.get(key, 0) >= val:
                continue
            eng.h.wait_ge(sem, val)
            eng.known[key] = val
            self.n_inst += 1

    def _deps(self, eng, reads, writes):
        for b in reads:
            self._need(eng, b.w)
        for b in writes:
            self._need(eng, b.w)
            self._need(eng, b.r)

    def op(self, eng, fn, reads=(), writes=(), flag=True):
        self._deps(eng, reads, writes)
        inst = fn()
        self.n_inst += 1
        if flag:
            eng.count += 1
            inst.then_inc(eng.sem, 1)
            val = eng.count
        else:
            val = eng.count + 1
        key = id(eng.sem)
        for b in reads:
            b.r[key] = (eng.sem, val)
        for b in writes:
            b.w = {key: (eng.sem, val)}
            b.r = {}
        return inst

    def opw(self, eng, fn, reads=(), writes=(), flag=True):
        self._deps(eng, reads, ())
        for b in writes:
            self._need(eng, b.r)
        inst = fn()
        self.n_inst += 1
        if flag:
            eng.count += 1
            inst.then_inc(eng.sem, 1)
            val = eng.count
        else:
            val = eng.count + 1
        key = id(eng.sem)
        for b in reads:
            b.r[key] = (eng.sem, val)
        for b in writes:
            b.w[key] = (eng.sem, val)
        return inst

    def dma(self, eng, out_buf, out_ap, in_buf, in_ap, owner=None, add=False, **kw):
        owner = owner or (out_buf if not hasattr(out_buf, "kind") else in_buf)
        if owner.dsem is None:
            owner.dsem = self.es.enter_context(self.nc.semaphore("dsem%d" % self.nsem))
            self.nsem += 1
        self._need(eng, in_buf.w)
        self._need(eng, out_buf.r)
        if not add:
            self._need(eng, out_buf.w)
        inst = eng.h.dma_start(out=out_ap, in_=in_ap, **kw)
        self.n_inst += 1
        owner.dcount += 16
        inst.then_inc(owner.dsem, 16)
        key = id(owner.dsem)
        tk = (owner.dsem, owner.dcount)
        in_buf.r[key] = tk
        if add:
            out_buf.w[key] = tk
        else:
            out_buf.w = {key: tk}
            out_buf.r = {}
        if getattr(out_buf, "kind", None) == "ExternalOutput":
            self.out_tickets.append(tk)
        return inst

    def barrier(self):
        tk = {}
        for e in self.engs:
            if e.count:
                tk[id(e.sem)] = (e.sem, e.count)
        for e in self.engs:
            self._need(e, {k: v for k, v in tk.items() if k != id(e.sem)})

    def finish(self):
        tk = {}
        for sem, val in self.out_tickets:
            k = id(sem)
            if k not in tk or tk[k][1] < val:
                tk[k] = (sem, val)
        self._need(self.sp, tk)
        self.es.close()
        return self.nc
D = 2048
SEQ = 16384
NCORE = 8
TPC = SEQ // NCORE
T = 512
NG = TPC // T
NCH = D // 128
DFF = 5632
NFC = DFF // 128
HD = 128
NH = 12
NKV = 4
NMH = 4
IH = 16
IDIM = 64
EPS = 1e-6
THETA = 10000.0
PI = float(np.pi)
TWO_PI = float(2 * np.pi)
C1 = 6.28125
C2 = float(2 * np.pi - 6.28125)
A_OFF = dict(q=0, k=1536, v=2048, qi=2560, ki=3584, wi=3648, qm=3664)


def core_tokens(c):
    idx = []
    for j in range(NG):
        s = (8 * j + c) * T
        idx.append(np.arange(s, s + T))
    return np.concatenate(idx)


def consts():
    ident = np.eye(128, dtype=np.float32)
    perm = np.zeros((128, 128), np.float32)
    for m in range(128):
        perm[(m + 64) % 128, m] = 1.0
    inv_h = (1.0 / (THETA ** (np.arange(0, HD, 2, dtype=np.float32) / HD))).astype(np.float32)
    inv_i = (1.0 / (THETA ** (np.arange(0, IDIM, 2, dtype=np.float32) / IDIM))).astype(np.float32)
    invh_col = np.concatenate([inv_h, inv_h]).reshape(128, 1).astype(np.float32)
    sgn_col = np.concatenate([-np.ones(64), np.ones(64)]).reshape(128, 1).astype(np.float32)
    invi_b = np.broadcast_to(inv_i[None, :], (128, 32)).astype(np.float32).copy()
    return dict(c_ident=ident, c_perm=perm, c_invh=invh_col, c_sgn=sgn_col, c_invi=invi_b)
def load_consts(kb, names_shapes):
    out = {}
    for name, shape, dt in names_shapes:
        d = kb.dram(name, shape, dt, kind="ExternalInput")
        s = kb.sb("s_" + name, shape, dt)
        kb.dma(kb.sp, s, s[:], d, d[:])
        out[name] = s
    return out


def load_gain(kb, name, n_layers=None):
    d = kb.dram(name, [D], F32, kind="ExternalInput")
    s = kb.sb("s_" + name, [128, NCH], F32)
    kb.dma(kb.sp, s, s[:], d, d.t.rearrange("(c p) -> p c", p=128), allow_slow_non_contiguous=True)
    return s


def emit_sin(kb, out_ap, ang_buf, ang_ap, shape, tmp, sgn_ap=None, phase=0.0):
    t0, ti, t1 = tmp
    n = shape
    kb.op(kb.dve, lambda: kb.nc.vector.tensor_scalar(t0[:, :n], ang_ap, float(phase), None, ALU.add),
          reads=[ang_buf], writes=[t0])
    kb.op(kb.dve, lambda: kb.nc.vector.tensor_scalar(t1[:, :n], t0[:, :n], 1.0 / TWO_PI, None, ALU.mult),
          reads=[t0], writes=[t1])
    kb.op(kb.dve, lambda: kb.nc.vector.tensor_copy(ti[:, :n], t1[:, :n]), reads=[t1], writes=[ti])
    kb.op(kb.dve, lambda: kb.nc.vector.tensor_copy(t1[:, :n], ti[:, :n]), reads=[ti], writes=[t1])
    kb.op(kb.dve, lambda: kb.nc.vector.scalar_tensor_tensor(t0[:, :n], t1[:, :n], -C1, t0[:, :n], ALU.mult, ALU.add),
          reads=[t1, t0], writes=[t0])
    kb.op(kb.dve, lambda: kb.nc.vector.scalar_tensor_tensor(t0[:, :n], t1[:, :n], -C2, t0[:, :n], ALU.mult, ALU.add),
          reads=[t1, t0], writes=[t0])
    kb.op(kb.dve, lambda: kb.nc.vector.tensor_scalar(t1[:, :n], t0[:, :n], PI, -TWO_PI, ALU.is_gt, ALU.mult),
          reads=[t0], writes=[t1])
    kb.op(kb.dve, lambda: kb.nc.vector.tensor_tensor(t0[:, :n], t0[:, :n], t1[:, :n], ALU.add),
          reads=[t0, t1], writes=[t0])
    kb.op(kb.dve, lambda: kb.nc.vector.tensor_scalar(t1[:, :n], t0[:, :n], -PI, TWO_PI, ALU.is_lt, ALU.mult),
          reads=[t0], writes=[t1])
    kb.op(kb.dve, lambda: kb.nc.vector.tensor_tensor(t0[:, :n], t0[:, :n], t1[:, :n], ALU.add),
          reads=[t0, t1], writes=[t0])
    kb.op(kb.dve, lambda: kb.nc.vector.tensor_scalar(t0[:, :n], t0[:, :n], PI, -PI, ALU.min, ALU.max),
          reads=[t0], writes=[t0])
    return t0


class Shared:
    pass


def setup_common(kb, S):
    cs = load_consts(kb, [("c_ident", [128, 128], F32), ("c_perm", [128, 128], F32),
                          ("c_invh", [128, 1], F32), ("c_sgn", [128, 1], F32), ("c_invi", [128, 32], F32)])
    S.ident = cs["c_ident"]; S.perm = cs["c_perm"]; S.invh = cs["c_invh"]; S.sgn = cs["c_sgn"]; S.invi = cs["c_invi"]
    S.identb = kb.sb("identb", [128, 128], BF16)
    kb.op(kb.dve, lambda: kb.nc.vector.tensor_copy(S.identb[:], S.ident[:]), reads=[S.ident], writes=[S.identb])
    S.onesb = kb.sb("onesb", [128, 128], BF16)
    kb.op(kb.dve, lambda: kb.nc.vector.memset(S.onesb[:], 1.0), writes=[S.onesb])
    S.t0 = kb.sb("sin_t0", [128, T], F32); S.ti = kb.sb("sin_ti", [128, T], I32); S.t1 = kb.sb("sin_t1", [128, T], F32)
    S.posi = kb.sb("posi", [128, T], I32)
    S.posf = kb.sb("posf", [128, T], F32)
    S.ropeC = kb.sb("ropeC", [128, T], F32)
    S.ropeS = kb.sb("ropeS", [128, T], F32)
    S.ang = kb.sb("ang", [128, T], F32)


def emit_rope_tables(kb, S, pos_d, g):
    nc = kb.nc
    kb.dma(kb.sp, S.posi, S.posi[:], pos_d, pos_d.t[g * T:(g + 1) * T].partition_broadcast(128))
    kb.op(kb.dve, lambda: nc.vector.tensor_copy(S.posf[:], S.posi[:]), reads=[S.posi], writes=[S.posf])
    kb.op(kb.dve, lambda: nc.vector.tensor_scalar(S.ang[:], S.posf[:], S.invh[:, 0:1], None, ALU.mult),
          reads=[S.posf, S.invh], writes=[S.ang])
    r = emit_sin(kb, None, S.ang, S.ang[:], T, (S.t0, S.ti, S.t1), phase=PI / 2)
    kb.op(kb.act, lambda: nc.scalar.activation(out=S.ropeC[:], in_=r[:, :T], func=AF.Sin), reads=[r], writes=[S.ropeC])
    r = emit_sin(kb, None, S.ang, S.ang[:], T, (S.t0, S.ti, S.t1), phase=0.0)
    kb.op(kb.act, lambda: nc.scalar.activation(out=S.ropeS[:], in_=r[:, :T], func=AF.Sin, scale=S.sgn[:, 0:1]),
          reads=[r, S.sgn], writes=[S.ropeS])


def emit_norm(kb, S, xT, gain, hT, sq, ps_ss, rstd):
    nc = kb.nc
    half = NCH // 2
    for hh in range(2):
        kb.opw(kb.act, lambda hh=hh: nc.scalar.activation(out=sq[:, hh * half:(hh + 1) * half, :],
                                                         in_=xT[:, hh * half:(hh + 1) * half, :], func=AF.Square),
               reads=[xT], writes=[sq])
    for c in range(NCH):
        kb.op(kb.pe, lambda c=c: nc.tensor.matmul(ps_ss[:], S.onesb[:], sq[:, c, :], start=(c == 0), stop=(c == NCH - 1)),
              reads=[S.onesb, sq], writes=[ps_ss], flag=(c == NCH - 1))
    kb.op(kb.act, lambda: nc.scalar.activation(out=rstd[:], in_=ps_ss[:], func=AF.Sqrt, scale=1.0 / D, bias=S.epsc[:, 0:1]),
          reads=[ps_ss, S.epsc], writes=[rstd])
    kb.op(kb.dve, lambda: nc.vector.reciprocal(rstd[:], rstd[:]), reads=[rstd], writes=[rstd])
    for c in range(NCH):
        kb.opw(kb.dve, lambda c=c: nc.vector.scalar_tensor_tensor(hT[:, c, :], xT[:, c, :], gain[:, c:c + 1], rstd[:],
                                                                 ALU.mult, ALU.mult),
               reads=[xT, gain, rstd], writes=[hT])


def emit_ffn(kb, S, xT, hT, act, wslots, wq, gu_d, down_d, ps_g, ps_u, ps_y, sil):
    nc = kb.nc
    NP = NFC // 2

    def next_slot():
        s = wslots[wq["i"] % len(wslots)]
        wq["i"] += 1
        return s

    gu_v = gu_d.t.rearrange("(k p) c -> p k c", p=128)
    for fp in range(NP):
        ws = next_slot()
        wv = ws.t[:, 0:NCH * 512].rearrange("p (k c) -> p k c", c=512)
        kb.dma(kb.pool, ws, wv[:, :, 0:256], gu_d, gu_v[:, :, fp * 256:(fp + 1) * 256])
        kb.dma(kb.pool, ws, wv[:, :, 256:512], gu_d, gu_v[:, :, DFF + fp * 256:DFF + (fp + 1) * 256], add=True)
        for j in range(2):
            f = fp * 2 + j
            pg = ps_g[f % 2]; pu = ps_u[f % 2]
            for k in range(NCH):
                kb.op(kb.pe, lambda k=k, j=j, pg=pg, wv=wv: nc.tensor.matmul(pg[:], wv[:, k, j * 128:(j + 1) * 128], hT[:, k, :],
                                                                      start=(k == 0), stop=(k == NCH - 1)),
                      reads=[ws, hT], writes=[pg], flag=(k == NCH - 1))
            for k in range(NCH):
                kb.op(kb.pe, lambda k=k, j=j, pu=pu, wv=wv: nc.tensor.matmul(pu[:], wv[:, k, 256 + j * 128:256 + (j + 1) * 128], hT[:, k, :],
                                                                      start=(k == 0), stop=(k == NCH - 1)),
                      reads=[ws, hT], writes=[pu], flag=(k == NCH - 1))
            sl = sil[f % 2]
            kb.op(kb.act, lambda pg=pg, sl=sl: nc.scalar.activation(out=sl[:], in_=pg[:], func=AF.Silu), reads=[pg], writes=[sl])
            kb.opw(kb.dve, lambda f=f, pu=pu, sl=sl: nc.vector.tensor_tensor(act[:, f, :], sl[:], pu[:], ALU.mult),
                   reads=[sl, pu], writes=[act])
    dn_v = down_d.t.rearrange("(f p) c -> p f c", p=128)
    for m in range(NCH):
        ws = next_slot()
        wv = ws.t[:, 0:NFC * 128].rearrange("p (f c) -> p f c", c=128)
        kb.dma(kb.pool, ws, wv[:, :, :], down_d, dn_v[:, :, m * 128:(m + 1) * 128])
        py = ps_y[m % 2]
        for f in range(NFC):
            kb.op(kb.pe, lambda f=f, py=py, wv=wv: nc.tensor.matmul(py[:], wv[:, f, :], act[:, f, :], start=(f == 0), stop=(f == NFC - 1)),
                  reads=[ws, act], writes=[py], flag=(f == NFC - 1))
        kb.opw(kb.dve, lambda m=m, py=py: nc.vector.scalar_tensor_tensor(xT[:, m, :], py[:], 0.5, xT[:, m, :], ALU.mult, ALU.add),
               reads=[py, xT], writes=[xT])


def emit_load_xT(kb, S, x_d, g, xT, xld, ps_t):
    nc = kb.nc
    n = 0
    for tt in range(T // 128):
        xl = xld[tt % 2]
        kb.dma(kb.sp, xl, xl[:], x_d, x_d.t[g * T + tt * 128:g * T + (tt + 1) * 128, :])
        for cq in range(4):
            pt = ps_t[n % 2]; n += 1
            for ci in range(4):
                c = cq * 4 + ci
                kb.op(kb.pe, lambda pt=pt, ci=ci, c=c, xl=xl: nc.tensor.transpose(pt[:, ci * 128:(ci + 1) * 128], xl[:, c * 128:(c + 1) * 128], S.ident[:]),
                      reads=[xl, S.ident], writes=[pt], flag=(ci == 3))
            eng = kb.act if (n % 2) else kb.dve
            if eng is kb.act:
                kb.opw(eng, lambda pt=pt, cq=cq, tt=tt: nc.scalar.copy(xT[:, cq * 4:cq * 4 + 4, tt * 128:(tt + 1) * 128],
                                                                pt.t[:, :].rearrange("p (c t) -> p c t", t=128)),
                       reads=[pt], writes=[xT])
            else:
                kb.opw(eng, lambda pt=pt, cq=cq, tt=tt: nc.vector.tensor_copy(xT[:, cq * 4:cq * 4 + 4, tt * 128:(tt + 1) * 128],
                                                                       pt.t[:, :].rearrange("p (c t) -> p c t", t=128)),
                       reads=[pt], writes=[xT])


def emit_store_xT(kb, xT, xs_d, g):
    kb.dma(kb.sp, xs_d, xs_d.t[:, :, g * T:(g + 1) * T], xT, xT[:, :, :])


def emit_proj_fm(kb, S, hT, w_d, col0, nchunks, rope, out_d, g, wslots, wq, pss, stg):
    nc = kb.nc
    psA, psB = pss
    w_v = w_d.t.rearrange("(k p) c -> p k c", p=128)
    ch = 0
    while ch < nchunks:
        nb = min(4, nchunks - ch)
        ws = wslots[wq["i"] % len(wslots)]; wq["i"] += 1
        wv = ws.t[:, 0:NCH * 512].rearrange("p (k c) -> p k c", c=512)
        kb.dma(kb.pool, ws, wv[:, :, 0:nb * 128], w_d, w_v[:, :, col0 + ch * 128:col0 + (ch + nb) * 128])
        for j in range(nb):
            cc = ch + j
            pa = psA[cc % 2]
            for k in range(NCH):
                kb.op(kb.pe, lambda k=k, j=j, pa=pa, wv=wv: nc.tensor.matmul(pa[:], wv[:, k, j * 128:(j + 1) * 128], hT[:, k, :],
                                                                      start=(k == 0), stop=(k == NCH - 1)),
                      reads=[ws, hT], writes=[pa], flag=(k == NCH - 1))
            ob = stg["ob"][cc % 2]
            if rope:
                a_sb = stg["a"][cc % 2]
                kb.op(kb.act, lambda pa=pa, a_sb=a_sb: nc.scalar.copy(a_sb[:], pa[:]), reads=[pa], writes=[a_sb])
                pb = psB[cc % 2]
                kb.op(kb.pe, lambda pb=pb, a_sb=a_sb: nc.tensor.matmul(pb[:], S.perm[:], a_sb[:], start=True, stop=True),
                      reads=[S.perm, a_sb], writes=[pb])
                t1 = stg["t1"][cc % 2]
                kb.op(kb.pool, lambda t1=t1, a_sb=a_sb: nc.gpsimd.tensor_tensor(t1[:], a_sb[:], S.ropeC[:], ALU.mult),
                      reads=[a_sb, S.ropeC], writes=[t1])
                t2 = stg["t2"][cc % 2]
                kb.op(kb.dve, lambda t2=t2, pb=pb: nc.vector.tensor_tensor(t2[:], pb[:], S.ropeS[:], ALU.mult),
                      reads=[pb, S.ropeS], writes=[t2])
                kb.op(kb.dve, lambda ob=ob, t1=t1, t2=t2: nc.vector.tensor_tensor(ob[:], t1[:], t2[:], ALU.add),
                      reads=[t1, t2], writes=[ob])
            else:
                kb.op(kb.act, lambda pa=pa, ob=ob: nc.scalar.copy(ob[:], pa[:]), reads=[pa], writes=[ob])
            kb.dma(kb.sp, out_d, out_d.t[cc, :, g * T:(g + 1) * T], ob, ob[:], add=True)
        ch += nb


def emit_idx_tables(kb, S, pos_d, g, I):
    nc = kb.nc
    for tt in range(4):
        kb.dma(kb.sp, I.pci, I.pci[:, tt:tt + 1], pos_d, pos_d.t[g * T + tt * 128:g * T + (tt + 1) * 128].rearrange("(p o) -> p o", o=1),
               add=(tt > 0), allow_slow_non_contiguous=True)
    kb.op(kb.dve, lambda: nc.vector.tensor_copy(I.pcf[:], I.pci[:]), reads=[I.pci], writes=[I.pcf])
    for tt in range(4):
        kb.opw(kb.dve, lambda tt=tt: nc.vector.tensor_scalar(I.ang[:, tt * 32:(tt + 1) * 32], S.invi[:], I.pcf[:, tt:tt + 1], None, ALU.mult),
               reads=[S.invi, I.pcf], writes=[I.ang])
    r = emit_sin(kb, None, I.ang, I.ang[:, 0:128], 128, (S.t0, S.ti, S.t1), phase=PI / 2)
    kb.op(kb.act, lambda: nc.scalar.activation(out=I.c[:], in_=r[:, :128], func=AF.Sin), reads=[r], writes=[I.c])
    r = emit_sin(kb, None, I.ang, I.ang[:, 0:128], 128, (S.t0, S.ti, S.t1), phase=0.0)
    kb.op(kb.act, lambda: nc.scalar.activation(out=I.s[:], in_=r[:, :128], func=AF.Sin), reads=[r], writes=[I.s])
    kb.op(kb.act, lambda: nc.scalar.mul(I.ns[:], I.s[:], -1.0), reads=[I.s], writes=[I.ns])


def rope_tm(kb, I, tt, nh, src, src_ap3, tmpA, tmpB, out, out_ap3):
    nc = kb.nc
    c_b = I.c[:, tt * 32:(tt + 1) * 32]
    s_b = I.s[:, tt * 32:(tt + 1) * 32]
    ns_b = I.ns[:, tt * 32:(tt + 1) * 32]
    bc = lambda ap: ap.unsqueeze(1).to_broadcast([128, nh, 32])
    A3 = tmpA.t[:, 0:nh * 64].rearrange("p (h d) -> p h d", d=64)
    B3 = tmpB.t[:, 0:nh * 64].rearrange("p (h d) -> p h d", d=64)
    kb.op(kb.dve, lambda: nc.vector.tensor_tensor(A3[:, :, 0:32], src_ap3[:, :, 0:32], bc(c_b), ALU.mult), reads=[src, I.c], writes=[tmpA])
    kb.opw(kb.dve, lambda: nc.vector.tensor_tensor(A3[:, :, 32:64], src_ap3[:, :, 32:64], bc(c_b), ALU.mult), reads=[src, I.c], writes=[tmpA])
    kb.op(kb.pool, lambda: nc.gpsimd.tensor_tensor(B3[:, :, 0:32], src_ap3[:, :, 32:64], bc(ns_b), ALU.mult), reads=[src, I.ns], writes=[tmpB])
    kb.opw(kb.pool, lambda: nc.gpsimd.tensor_tensor(B3[:, :, 32:64], src_ap3[:, :, 0:32], bc(s_b), ALU.mult), reads=[src, I.s], writes=[tmpB])
    kb.op(kb.dve, lambda: nc.vector.tensor_tensor(out_ap3, A3, B3, ALU.add), reads=[tmpA, tmpB], writes=[out])


def emit_proj_tm_a(kb, S, I, hT, w_d, g, wslots, wq, ps_list, stg, outs):
    nc = kb.nc
    v_d, qi_d, kiT_d, wi_d = outs
    w_v = w_d.t.rearrange("(k p) c -> p k c", p=128)
    blocks = [("v", A_OFF["v"], 512), ("qi0", A_OFF["qi"], 512), ("qi1", A_OFF["qi"] + 512, 512), ("kw", A_OFF["ki"], 80)]
    n = 0
    for name, c0, ncol in blocks:
        ws = wslots[wq["i"] % len(wslots)]; wq["i"] += 1
        wv = ws.t[:, 0:NCH * 512].rearrange("p (k c) -> p k c", c=512)
        kb.dma(kb.pool, ws, wv[:, :, 0:ncol], w_d, w_v[:, :, c0:c0 + ncol])
        for tt in range(4):
            ps = ps_list[n % 2]; n += 1
            for k in range(NCH):
                kb.op(kb.pe, lambda k=k, ps=ps, wv=wv, tt=tt, ncol=ncol: nc.tensor.matmul(ps[:, 0:ncol], hT[:, k, tt * 128:(tt + 1) * 128], wv[:, k, 0:ncol],
                                                                                start=(k == 0), stop=(k == NCH - 1)),
                      reads=[ws, hT], writes=[ps], flag=(k == NCH - 1))
            tok0 = g * T + tt * 128
            if name == "v":
                ob = stg["ob"][n % 2]
                kb.op(kb.act, lambda ps=ps, ob=ob: nc.scalar.copy(ob[:], ps[:]), reads=[ps], writes=[ob])
                kb.dma(kb.sp, v_d, v_d.t[tok0:tok0 + 128, :], ob, ob[:], add=True)
            elif name in ("qi0", "qi1"):
                a_sb = stg["a"][n % 2]
                kb.op(kb.act, lambda ps=ps, a_sb=a_sb: nc.scalar.copy(a_sb[:], ps[:]), reads=[ps], writes=[a_sb])
                ob = stg["ob"][n % 2]
                rope_tm(kb, I, tt, 8, a_sb, a_sb.t[:, :].rearrange("p (h d) -> p h d", d=64), stg["t1"][n % 2], stg["t2"][n % 2],
                        ob, ob.t[:, :].rearrange("p (h d) -> p h d", d=64))
                hc = 0 if name == "qi0" else 512
                kb.dma(kb.sp, qi_d, qi_d.t[tok0:tok0 + 128, hc:hc + 512], ob, ob[:], add=True)
            else:
                a_sb = stg["a"][n % 2]
                kb.op(kb.act, lambda ps=ps, a_sb=a_sb: nc.scalar.copy(a_sb[:, 0:80], ps[:, 0:80]), reads=[ps], writes=[a_sb])
                wo = I.wo[tt % 2]
                kb.op(kb.act, lambda wo=wo, a_sb=a_sb: nc.scalar.mul(wo[:], a_sb[:, 64:80], float((IH ** -0.5) * (IDIM ** -0.5))), reads=[a_sb], writes=[wo])
                kb.dma(kb.sp, wi_d, wi_d.t[tok0:tok0 + 128, :], wo, wo[:], add=True)
                kb.op(kb.act, lambda a_sb=a_sb: nc.scalar.activation(out=I.junk[:], in_=a_sb[:, 0:64], func=AF.Square, accum_out=I.kss[:, 0:1]),
                      reads=[a_sb], writes=[I.junk, I.kss])
                kb.op(kb.act, lambda: nc.scalar.activation(out=I.krs[:], in_=I.kss[:], func=AF.Sqrt, scale=1.0 / IDIM, bias=S.epsc[:, 0:1]),
                      reads=[I.kss, S.epsc], writes=[I.krs])
                kb.op(kb.dve, lambda: nc.vector.reciprocal(I.krs[:], I.krs[:]), reads=[I.krs], writes=[I.krs])
                kb.op(kb.dve, lambda a_sb=a_sb: nc.vector.scalar_tensor_tensor(I.kn[:], a_sb[:, 0:64], I.krs[:, 0:1], I.kgain[:], ALU.mult, ALU.mult),
                      reads=[a_sb, I.krs, I.kgain], writes=[I.kn])
                rope_tm(kb, I, tt, 1, I.kn, I.kn.t[:, :].rearrange("p (h d) -> p h d", d=64), stg["t1"][n % 2], stg["t2"][n % 2],
                        I.kr, I.kr.t[:, :].rearrange("p (h d) -> p h d", d=64))
                kb.op(kb.pe, lambda: nc.tensor.transpose(I.ps_kt[0:64, 0:128], I.kr[:, 0:64], S.identb[:]), reads=[I.kr, S.identb], writes=[I.ps_kt])
                kb.opw(kb.act, lambda tt=tt: nc.scalar.copy(I.kiT[0:64, tt * 128:(tt + 1) * 128], I.ps_kt[0:64, 0:128]), reads=[I.ps_kt], writes=[I.kiT])
    kb.dma(kb.sp, kiT_d, kiT_d.t[:, g * T:(g + 1) * T], I.kiT, I.kiT[0:64, :], add=True)


def alloc_ffn_state(kb, S, nslots=3):
    S.xT = kb.sb("xT", [128, NCH, T], F32)
    S.hT = kb.sb("hT", [128, NCH, T], BF16)
    S.act = kb.sb("act", [128, NFC, T], BF16)
    S.wslots = [kb.sb("wslot%d" % i, [128, NCH * 512], BF16) for i in range(nslots)]
    S.wq = {"i": 0}
    S.ps_g = [kb.ps("ps_g%d" % i, [128, T]) for i in range(2)]
    S.ps_u = [kb.ps("ps_u%d" % i, [128, T]) for i in range(2)]
    S.ps_y = [kb.ps("ps_y%d" % i, [128, T]) for i in range(2)]
    S.ps_ss = kb.ps("ps_ss", [128, T])
    S.sil = [kb.sb("sil%d" % i, [128, T], BF16) for i in range(2)]
    S.rstd = kb.sb("rstd", [128, T], F32)
    S.epsc = kb.sb("epsc", [128, 1], F32)
    kb.op(kb.dve, lambda: kb.nc.vector.memset(S.epsc[:], EPS), writes=[S.epsc])


def build_a(ng=NG):
    kb = KB()
    nc = kb.nc
    S = Shared()
    setup_common(kb, S)
    alloc_ffn_state(kb, S)
    x_d = kb.dram("x", [TPC, D], F32, kind="ExternalInput")
    pos_d = kb.dram("pos", [TPC], I32, kind="ExternalInput")
    gu_d = kb.dram("w_gu", [D, 2 * DFF], F32, kind="ExternalInput")
    dn_d = kb.dram("w_dn", [DFF, D], F32, kind="ExternalInput")
    win_d = kb.dram("w_in", [D, 4176], F32, kind="ExternalInput")
    g_ffn = load_gain(kb, "g_ffn")
    g_att = load_gain(kb, "g_att")
    kg_d = kb.dram("g_idxk", [IDIM], F32, kind="ExternalInput")
    I = Shared()
    I.kgain = kb.sb("kgain", [128, IDIM], F32)
    kb.dma(kb.sp, I.kgain, I.kgain[:], kg_d, kg_d.t.partition_broadcast(128))
    I.pci = kb.sb("pci", [128, 4], I32); I.pcf = kb.sb("pcf", [128, 4], F32)
    I.ang = kb.sb("iang", [128, 128], F32)
    I.c = kb.sb("ic", [128, 128], F32); I.s = kb.sb("is", [128, 128], F32); I.ns = kb.sb("ins", [128, 128], F32)
    I.wo = [kb.sb("wo%d" % i, [128, 16], F32) for i in range(2)]
    I.junk = kb.sb("junk", [128, 64], F32); I.kss = kb.sb("kss", [128, 1], F32); I.krs = kb.sb("krs", [128, 1], F32)
    I.kn = kb.sb("kn", [128, 64], F32); I.kr = kb.sb("kr", [128, 64], BF16)
    I.ps_kt = kb.ps("ps_kt", [128, 128], BF16)
    I.kiT = kb.sb("kiT", [64, T], BF16)
    xld = [kb.sb("xld%d" % i, [128, D], F32) for i in range(2)]
    stg = dict(ob=[kb.sb("ob%d" % i, [128, T], BF16) for i in range(2)],
               a=[kb.sb("a%d" % i, [128, T], F32) for i in range(2)],
               t1=[kb.sb("t1%d" % i, [128, T], F32) for i in range(2)],
               t2=[kb.sb("t2%d" % i, [128, T], F32) for i in range(2)])
    x1_d = kb.dram("o_x1T", [128, NCH, TPC], F32, kind="ExternalOutput")
    qT_d = kb.dram("o_qT", [NH, 128, TPC], BF16, kind="ExternalOutput")
    kT_d = kb.dram("o_kT", [NKV, 128, TPC], BF16, kind="ExternalOutput")
    qmT_d = kb.dram("o_qmT", [NMH, 128, TPC], BF16, kind="ExternalOutput")
    v_d = kb.dram("o_v", [TPC, NKV * HD], BF16, kind="ExternalOutput")
    qi_d = kb.dram("o_qi", [TPC, IH * IDIM], BF16, kind="ExternalOutput")
    kiT_d = kb.dram("o_kiT", [IDIM, TPC], BF16, kind="ExternalOutput")
    wi_d = kb.dram("o_wi", [TPC, IH], F32, kind="ExternalOutput")
    for g in range(ng):
        emit_load_xT(kb, S, x_d, g, S.xT, xld, S.ps_y)
        emit_rope_tables(kb, S, pos_d, g)
        emit_idx_tables(kb, S, pos_d, g, I)
        emit_norm(kb, S, S.xT, g_ffn, S.hT, S.act, S.ps_ss, S.rstd)
        emit_ffn(kb, S, S.xT, S.hT, S.act, S.wslots, S.wq, gu_d, dn_d, S.ps_g, S.ps_u, S.ps_y, S.sil)
        emit_store_xT(kb, S.xT, x1_d, g)
        emit_norm(kb, S, S.xT, g_att, S.hT, S.act, S.ps_ss, S.rstd)
        emit_proj_fm(kb, S, S.hT, win_d, A_OFF["q"], NH, True, qT_d, g, S.wslots, S.wq, (S.ps_g, S.ps_u), stg)
        emit_proj_fm(kb, S, S.hT, win_d, A_OFF["k"], NKV, True, kT_d, g, S.wslots, S.wq, (S.ps_g, S.ps_u), stg)
        emit_proj_fm(kb, S, S.hT, win_d, A_OFF["qm"], NMH, False, qmT_d, g, S.wslots, S.wq, (S.ps_g, S.ps_u), stg)
        emit_proj_tm_a(kb, S, I, S.hT, win_d, g, S.wslots, S.wq, S.ps_y, stg, (v_d, qi_d, kiT_d, wi_d))
    nc = kb.finish()
    return nc, kb


def alloc_ffn_state_nb(kb, S, nslots=3):
    S.xT = kb.sb("xT", [128, NCH, T], F32)
    S.hT = kb.sb("hT", [128, NCH, T], BF16)
    S.act = kb.sb("act", [128, NFC, T], BF16)
    S.wslots = [kb.sb("wslot%d" % i, [128, NCH * 512], BF16) for i in range(nslots)]
    S.wq = {"i": 0}
    S.ps_g = [kb.ps("ps_g%d" % i, [128, T]) for i in range(2)]
    S.ps_u = [kb.ps("ps_u%d" % i, [128, T]) for i in range(2)]
    S.ps_y = [kb.ps("ps_y%d" % i, [128, T]) for i in range(2)]
    S.ps_ss = kb.ps("ps_ss", [128, T])
    S.sil = [kb.sb("sil%d" % i, [128, T], BF16) for i in range(2)]
    S.rstd = kb.sb("rstd", [128, T], F32)


NBIS = 20
BIG = 1.0e30


def uniform_extent(j):
    return 8 * j + 8


def emit_causal_mask(kb, X, qt, st_global, out):
    nc = kb.nc
    kb.op(kb.dve, lambda: nc.vector.tensor_scalar(out[:], X.qposB[:, qt * 128:(qt + 1) * 128], float(st_global * 128), X.iota[:, 0:1],
                                                   ALU.subtract, ALU.is_ge),
          reads=[X.qposB, X.iota], writes=[out])


def emit_bcast_row(kb, X, col, out_ps):
    nc = kb.nc
    kb.op(kb.dve, lambda: nc.vector.tensor_scalar(X.diag[:], X.identf[:], col[:, 0:1], None, ALU.mult), reads=[X.identf, col], writes=[X.diag])
    kb.op(kb.pe, lambda: nc.tensor.matmul(out_ps[:], X.onesf[:], X.diag[:], start=True, stop=True), reads=[X.onesf, X.diag], writes=[out_ps])


def emit_indexer_qtile(kb, S, X, qt, j, qi_d, mask_d):
    nc = kb.nc
    U = uniform_extent(j)
    E = U * 4
    qv = qi_d.t.rearrange("(g t) (h d) -> g (t h) d", t=8, d=IDIM)
    kb.dma(kb.sp, X.qrows, X.qrows.t[:, :].rearrange("p (g d) -> p g d", d=IDIM), qi_d,
           qv[qt * 16:(qt + 1) * 16].rearrange("g p d -> p g d"))
    for gg in range(16):
        kb.op(kb.pe, lambda gg=gg: nc.tensor.transpose(X.ps_qt[0:64, gg * 128:(gg + 1) * 128], X.qrows[:, gg * 64:(gg + 1) * 64], S.identb[:]),
              reads=[X.qrows, S.identb], writes=[X.ps_qt], flag=(gg == 15))
    kb.op(kb.act, lambda: nc.scalar.copy(X.qgT[0:64, :], X.ps_qt[0:64, :]), reads=[X.ps_qt], writes=[X.qgT])
    for gg in range(16):
        grp = qt * 16 + gg
        kb.opw(kb.dve, lambda gg=gg, grp=grp: nc.vector.tensor_scalar(X.wg[:, gg * 8:(gg + 1) * 8], X.E[:], X.wT[:, grp:grp + 1], None, ALU.mult),
               reads=[X.E, X.wT], writes=[X.wg])
    n = 0
    for u in range(U):
        ip = X.ps_isc[u % 2]
        for gg in range(16):
            pd = X.ps_dots[n % 2]
            kb.op(kb.pe, lambda gg=gg, pd=pd, u=u: nc.tensor.matmul(pd[:], X.qgT[0:64, gg * 128:(gg + 1) * 128], X.kiT[0:64, u * 512:(u + 1) * 512],
                                                                start=True, stop=True),
                  reads=[X.qgT, X.kiT], writes=[pd])
            R = X.R[n % 2]
            if n % 2 == 0:
                kb.op(kb.act, lambda pd=pd, R=R: nc.scalar.activation(out=R[:], in_=pd[:], func=AF.Relu), reads=[pd], writes=[R])
            else:
                kb.op(kb.dve, lambda pd=pd, R=R: nc.vector.tensor_scalar(R[:], pd[:], 0.0, None, ALU.max), reads=[pd], writes=[R])
            n += 1
            for st in range(4):
                kb.op(kb.pe, lambda st=st, gg=gg, ip=ip, R=R: nc.tensor.matmul(ip[:, st * 128 + gg * 8:st * 128 + (gg + 1) * 8], R[:, st * 128:(st + 1) * 128],
                                                                         X.wg[:, gg * 8:(gg + 1) * 8], start=True, stop=True),
                      reads=[R, X.wg], writes=[ip], flag=(st == 3))
        kb.opw(kb.act, lambda u=u, ip=ip: nc.scalar.copy(X.iscT[:, u * 512:(u + 1) * 512], ip[:]), reads=[ip], writes=[X.iscT])
    iv = X.iscT.t[:, 0:E * 128].rearrange("p (e t) -> p t e", t=128)
    kb.op(kb.dve, lambda: nc.vector.tensor_reduce(X.pmax[:], iv, AX.X, ALU.max), reads=[X.iscT], writes=[X.pmax])
    kb.op(kb.dve, lambda: nc.vector.tensor_reduce(X.pmin[:], iv, AX.X, ALU.min), reads=[X.iscT], writes=[X.pmin])
    for (src, op, dst) in ((X.pmax, ALU.max, X.hi), (X.pmin, ALU.min, X.lo)):
        kb.op(kb.pe, lambda src=src: nc.tensor.transpose(X.ps_sm[:], src[:], X.identf[:]), reads=[src, X.identf], writes=[X.ps_sm])
        kb.op(kb.dve, lambda op=op: nc.vector.tensor_reduce(X.col[:], X.ps_sm[:], AX.X, op), reads=[X.ps_sm], writes=[X.col])
        emit_bcast_row(kb, X, X.col, X.ps_sm)
        if dst is X.hi:
            kb.op(kb.dve, lambda: nc.vector.tensor_scalar(X.hi[:], X.ps_sm[:], 1.0, None, ALU.add), reads=[X.ps_sm], writes=[X.hi])
        else:
            kb.op(kb.dve, lambda: nc.vector.tensor_scalar(X.lo[:], X.ps_sm[:], -1.0, None, ALU.add), reads=[X.ps_sm], writes=[X.lo])
    for u in range(8 * j, U):
        for st in range(4):
            sg = u * 4 + st
            emit_causal_mask(kb, X, qt, sg, X.cm)
            kb.op(kb.dve, lambda: nc.vector.tensor_scalar(X.cmb[:], X.cm[:], BIG, -BIG, ALU.mult, ALU.add), reads=[X.cm], writes=[X.cmb])
            sl = X.iscT[:, sg * 128:(sg + 1) * 128]
            kb.op(kb.dve, lambda sl=sl: nc.vector.tensor_tensor(sl, sl, X.cm[:], ALU.mult), reads=[X.iscT, X.cm], writes=[X.iscT])
            kb.op(kb.dve, lambda sl=sl: nc.vector.tensor_tensor(sl, sl, X.cmb[:], ALU.add), reads=[X.iscT, X.cmb], writes=[X.iscT])
    i3 = X.iscT.t[:, 0:E * 128].rearrange("p (e t) -> p e t", t=128)
    d3 = X.ind.t[:, 0:E * 128].rearrange("p (e t) -> p e t", t=128)
    NPC = 16

    def compare(thr):
        pieces = []
        e0 = 0
        while e0 < E:
            e1 = min(E, e0 + NPC)
            pieces.append((e0, e1))
            e0 = e1
        for pi, (e0, e1) in enumerate(pieces):
            eng = kb.dve
            h = nc.vector
            kb.opw(eng, lambda e0=e0, e1=e1, h=h: h.tensor_tensor(d3[:, e0:e1, :], i3[:, e0:e1, :], thr[:].unsqueeze(1).to_broadcast([128, e1 - e0, 128]), ALU.is_ge),
                   reads=[X.iscT, thr], writes=[X.ind])
        return pieces

    for it in range(NBIS):
        kb.op(kb.dve, lambda: nc.vector.tensor_tensor(X.mid[:], X.lo[:], X.hi[:], ALU.add), reads=[X.lo, X.hi], writes=[X.mid])
        kb.op(kb.dve, lambda: nc.vector.tensor_scalar(X.mid[:], X.mid[:], 0.5, None, ALU.mult), reads=[X.mid], writes=[X.mid])
        compare(X.mid)
        nmm = E // 4
        for q in range(nmm):
            kb.op(kb.pe, lambda q=q: nc.tensor.matmul(X.ps_cnt[:], S.onesb[:], X.ind[:, q * 512:(q + 1) * 512], start=(q == 0), stop=(q == nmm - 1)),
                  reads=[S.onesb, X.ind], writes=[X.ps_cnt], flag=(q == nmm - 1))
        kb.op(kb.dve, lambda: nc.vector.tensor_reduce(X.c1[:], X.ps_cnt.t[:, :].rearrange("p (j t) -> p t j", t=128), AX.X, ALU.add),
              reads=[X.ps_cnt], writes=[X.c1])
        kb.op(kb.dve, lambda: nc.vector.tensor_scalar(X.sel[:], X.c1[:], 255.5, None, ALU.is_ge), reads=[X.c1], writes=[X.sel])
        kb.op(kb.dve, lambda: nc.vector.tensor_tensor(X.d1[:], X.mid[:], X.lo[:], ALU.subtract), reads=[X.mid, X.lo], writes=[X.d1])
        kb.op(kb.dve, lambda: nc.vector.tensor_tensor(X.d1[:], X.d1[:], X.sel[:], ALU.mult), reads=[X.d1, X.sel], writes=[X.d1])
        kb.op(kb.dve, lambda: nc.vector.tensor_tensor(X.lo[:], X.lo[:], X.d1[:], ALU.add), reads=[X.lo, X.d1], writes=[X.lo])
        kb.op(kb.dve, lambda: nc.vector.tensor_tensor(X.d1[:], X.hi[:], X.mid[:], ALU.subtract), reads=[X.hi, X.mid], writes=[X.d1])
        kb.op(kb.dve, lambda: nc.vector.tensor_tensor(X.d1[:], X.d1[:], X.sel[:], ALU.mult), reads=[X.d1, X.sel], writes=[X.d1])
        kb.op(kb.dve, lambda: nc.vector.tensor_tensor(X.hi[:], X.mid[:], X.d1[:], ALU.add), reads=[X.mid, X.d1], writes=[X.hi])
    compare(X.lo)
    kb.dma(kb.sp, mask_d, mask_d.t[qt, :, 0:E * 128], X.ind, X.ind[:, 0:E * 128])


SCALE = float(HD ** -0.5)


def emit_stab(kb, S, Y, qT_src, nq_cols):
    nc = kb.nc
    first = True
    for (src, ncols, dst) in ((Y.Kg, SEQ, Y.kmx), (qT_src, nq_cols, Y.qmx)):
        nch = ncols // 512
        for i in range(nch):
            sq = Y.sq[i % 2]
            kb.op(kb.act, lambda sq=sq, src=src, i=i: nc.scalar.activation(out=sq[:], in_=src[:, i * 512:(i + 1) * 512], func=AF.Square), reads=[src], writes=[sq])
            pn = Y.ps_n[i % 2]
            kb.op(kb.pe, lambda pn=pn, sq=sq: nc.tensor.matmul(pn[:], S.onesb[:], sq[:], start=True, stop=True), reads=[S.onesb, sq], writes=[pn])
            kb.opw(kb.dve, lambda pn=pn, i=i: nc.vector.tensor_reduce(Y.nrm[:, i:i + 1], pn[:], AX.X, ALU.max), reads=[pn], writes=[Y.nrm])
        kb.op(kb.dve, lambda dst=dst, nch=nch: nc.vector.tensor_reduce(dst[:], Y.nrm[:, 0:nch], AX.X, ALU.max), reads=[Y.nrm], writes=[dst])
    kb.op(kb.dve, lambda: nc.vector.tensor_tensor(Y.kmx[:], Y.kmx[:], Y.qmx[:], ALU.mult), reads=[Y.kmx, Y.qmx], writes=[Y.kmx])
    kb.op(kb.act, lambda: nc.scalar.activation(out=Y.negM[:], in_=Y.kmx[:], func=AF.Sqrt), reads=[Y.kmx], writes=[Y.negM])
    kb.op(kb.act, lambda: nc.scalar.mul(Y.negM[:], Y.negM[:], -SCALE), reads=[Y.negM], writes=[Y.negM])


def emit_attn_qtile(kb, S, Y, qt, j, gq, mode, oT_d, head0, X=None):
    nc = kb.nc
    U = uniform_extent(j)
    E = U * 4
    q3 = Y.qall[:, qt * 384:(qt + 1) * 384]
    for e in range(E):
        pl = Y.ps_l[e % 2]
        kb.op(kb.pe, lambda e=e, pl=pl: nc.tensor.matmul(pl[:, 0:384], Y.Kg[:, e * 128:(e + 1) * 128], q3, start=True, stop=(mode != "moba")),
              reads=[Y.Kg, Y.qall], writes=[pl], flag=(mode != "moba"))
        if mode == "moba":
            n = e // 2
            kb.op(kb.pe, lambda pl=pl, n=n: nc.tensor.matmul(pl[:, 0:384], Y.En[0:64, n * 128:(n + 1) * 128], Y.biasT[0:64, :], start=False, stop=True),
                  reads=[Y.En, Y.biasT], writes=[pl])
        pt = Y.pT[e % 2]
        kb.op(kb.act, lambda pt=pt, pl=pl: nc.scalar.activation(out=pt[:], in_=pl[:, 0:384], func=AF.Exp, scale=SCALE, bias=Y.negM[:, 0:1]),
              reads=[pl, Y.negM], writes=[pt])
        p3 = pt.t[:, :].rearrange("p (h t) -> p h t", t=128)
        if mode == "dsa":
            mk = Y.mask[:, e * 128:(e + 1) * 128].unsqueeze(1).to_broadcast([128, 3, 128])
            eng = kb.dve if e % 2 == 0 else kb.pool
            h = nc.vector if eng is kb.dve else nc.gpsimd
            kb.op(eng, lambda h=h, p3=p3, mk=mk: h.tensor_tensor(p3, p3, mk, ALU.mult), reads=[pt, Y.mask], writes=[pt])
        elif e >= 8 * j * 4:
            emit_causal_mask(kb, Y, qt, e, Y.cm)
            kb.op(kb.dve, lambda p3=p3: nc.vector.tensor_tensor(p3, p3, Y.cm[:].unsqueeze(1).to_broadcast([128, 3, 128]), ALU.mult),
                  reads=[pt, Y.cm], writes=[pt])
        kb.op(kb.pe, lambda e=e, pt=pt: nc.tensor.matmul(Y.ps_o[:, 0:384], Y.Vg[:, e * 128:(e + 1) * 128], pt[:], start=(e == 0), stop=(e == E - 1)),
              reads=[Y.Vg, pt], writes=[Y.ps_o], flag=False)
        kb.op(kb.pe, lambda e=e, pt=pt: nc.tensor.matmul(Y.ps_r[:, 0:384], S.onesb[:], pt[:], start=(e == 0), stop=(e == E - 1)),
              reads=[S.onesb, pt], writes=[Y.ps_r], flag=True)
    kb.op(kb.dve, lambda: nc.vector.reciprocal(Y.rinv[:], Y.ps_r[:, 0:384]), reads=[Y.ps_r], writes=[Y.rinv])
    os_ = Y.ostg[qt % 2]
    kb.op(kb.dve, lambda os_=os_: nc.vector.tensor_tensor(os_[:], Y.ps_o[:, 0:384], Y.rinv[:], ALU.mult), reads=[Y.ps_o, Y.rinv], writes=[os_])
    kb.dma(kb.sp, oT_d, oT_d.t[head0:head0 + 3, :, qt * 128:(qt + 1) * 128].rearrange("h p t -> p h t"), os_,
           os_.t[:, :].rearrange("p (h t) -> p h t", t=128), add=True)


def emit_mem_kv(kb, S, M, mem_d, g_mem, wmem_d):
    nc = kb.nc
    for mt in range(2):
        kb.dma(kb.sp, M.xld, M.xld[:], mem_d, mem_d.t[mt * 128:(mt + 1) * 128, :])
        for cq in range(4):
            for ci in range(4):
                c = cq * 4 + ci
                kb.op(kb.pe, lambda ci=ci, c=c: nc.tensor.transpose(M.ps_a[:, ci * 128:(ci + 1) * 128], M.xld[:, c * 128:(c + 1) * 128], S.ident[:]),
                      reads=[M.xld, S.ident], writes=[M.ps_a], flag=(ci == 3))
            kb.opw(kb.act, lambda cq=cq, mt=mt: nc.scalar.copy(M.memT[:, cq * 4:cq * 4 + 4, mt * 128:(mt + 1) * 128],
                                                         M.ps_a.t[:, :].rearrange("p (c t) -> p c t", t=128)), reads=[M.ps_a], writes=[M.memT])
    kb.op(kb.act, lambda: nc.scalar.activation(out=M.sq[:], in_=M.memT[:], func=AF.Square), reads=[M.memT], writes=[M.sq])
    for c in range(NCH):
        kb.op(kb.pe, lambda c=c: nc.tensor.matmul(M.ps_b[:, 0:256], S.onesb[:], M.sq[:, c, :], start=(c == 0), stop=(c == NCH - 1)),
              reads=[S.onesb, M.sq], writes=[M.ps_b], flag=(c == NCH - 1))
    kb.op(kb.act, lambda: nc.scalar.activation(out=M.rstd[:], in_=M.ps_b[:, 0:256], func=AF.Sqrt, scale=1.0 / D, bias=S.epsc[:, 0:1]),
          reads=[M.ps_b, S.epsc], writes=[M.rstd])
    kb.op(kb.dve, lambda: nc.vector.reciprocal(M.rstd[:], M.rstd[:]), reads=[M.rstd], writes=[M.rstd])
    for c in range(NCH):
        kb.opw(kb.dve, lambda c=c: nc.vector.scalar_tensor_tensor(M.hm[:, c, :], M.memT[:, c, :], g_mem[:, c:c + 1], M.rstd[:], ALU.mult, ALU.mult),
               reads=[M.memT, g_mem, M.rstd], writes=[M.hm])
    w_v = wmem_d.t.rearrange("(k p) c -> p k c", p=128)
    wv = M.w.t[:, :].rearrange("p (k c) -> p k c", c=512)
    kb.dma(kb.pool, M.w, wv[:, :, :], wmem_d, w_v[:, :, 0:512])
    for h in range(NMH):
        for k in range(NCH):
            kb.op(kb.pe, lambda h=h, k=k: nc.tensor.matmul(M.ps_a[:, 0:256], wv[:, k, h * 128:(h + 1) * 128], M.hm[:, k, :], start=(k == 0), stop=(k == NCH - 1)),
                  reads=[M.w, M.hm], writes=[M.ps_a], flag=(k == NCH - 1))
        kb.opw(kb.act, lambda h=h: nc.scalar.copy(M.kmT[:, h * 256:(h + 1) * 256], M.ps_a[:, 0:256]), reads=[M.ps_a], writes=[M.kmT])
    kb.dma(kb.pool, M.w, wv[:, :, :], wmem_d, w_v[:, :, 512:1024])
    for mt in range(2):
        for k in range(NCH):
            kb.op(kb.pe, lambda mt=mt, k=k: nc.tensor.matmul(M.ps_b[:], M.hm[:, k, mt * 128:(mt + 1) * 128], wv[:, k, :], start=(k == 0), stop=(k == NCH - 1)),
                  reads=[M.w, M.hm], writes=[M.ps_b], flag=(k == NCH - 1))
        kb.opw(kb.act, lambda mt=mt: nc.scalar.copy(M.vm[:, mt * 512:(mt + 1) * 512], M.ps_b[:]), reads=[M.ps_b], writes=[M.vm])


def emit_mem_attn(kb, S, M, Y, qmT_d, oT_d, nqt):
    nc = kb.nc
    for h in range(NMH):
        kb.dma(kb.sp, Y.qm, Y.qm[:], qmT_d, qmT_d.t[h, :, :])
        kb.op(kb.act, lambda h=h: nc.scalar.copy(Y.Kg[:, 0:256], M.kmT[:, h * 256:(h + 1) * 256]), reads=[M.kmT], writes=[Y.Kg])
        first = True
        for (src, ncols, dst) in ((Y.Kg, 256, Y.kmx), (Y.qm, TPC, Y.qmx)):
            nch = max(1, ncols // 512)
            w = min(512, ncols)
            for i in range(nch):
                sq = Y.sq[i % 2]
                kb.op(kb.act, lambda sq=sq, src=src, i=i, w=w: nc.scalar.activation(out=sq[:, 0:w], in_=src[:, i * w:(i + 1) * w], func=AF.Square), reads=[src], writes=[sq])
                pn = Y.ps_n[i % 2]
                kb.op(kb.pe, lambda pn=pn, sq=sq, w=w: nc.tensor.matmul(pn[:, 0:w], S.onesb[:], sq[:, 0:w], start=True, stop=True), reads=[S.onesb, sq], writes=[pn])
                kb.opw(kb.dve, lambda pn=pn, i=i, w=w: nc.vector.tensor_reduce(Y.nrm[:, i:i + 1], pn[:, 0:w], AX.X, ALU.max), reads=[pn], writes=[Y.nrm])
            kb.op(kb.dve, lambda dst=dst, nch=nch: nc.vector.tensor_reduce(dst[:], Y.nrm[:, 0:nch], AX.X, ALU.max), reads=[Y.nrm], writes=[dst])
        kb.op(kb.dve, lambda: nc.vector.tensor_tensor(Y.kmx[:], Y.kmx[:], Y.qmx[:], ALU.mult), reads=[Y.kmx, Y.qmx], writes=[Y.kmx])
        kb.op(kb.act, lambda: nc.scalar.activation(out=Y.negM[:], in_=Y.kmx[:], func=AF.Sqrt), reads=[Y.kmx], writes=[Y.negM])
        kb.op(kb.act, lambda: nc.scalar.mul(Y.negM[:], Y.negM[:], -SCALE), reads=[Y.negM], writes=[Y.negM])
        for qc in range(TPC // 384 + 1):
            t0 = qc * 384
            w = min(384, TPC - t0)
            if w <= 0:
                break
            for mt in range(2):
                pl = Y.ps_l[mt % 2]
                kb.op(kb.pe, lambda pl=pl, mt=mt, h=h, t0=t0, w=w: nc.tensor.matmul(pl[:, 0:w], M.kmT[:, h * 256 + mt * 128:h * 256 + (mt + 1) * 128], Y.qm[:, t0:t0 + w],
                                                                             start=True, stop=True), reads=[M.kmT, Y.qm], writes=[pl])
                pt = Y.pT[mt % 2]
                kb.op(kb.act, lambda pt=pt, pl=pl, w=w: nc.scalar.activation(out=pt[:, 0:w], in_=pl[:, 0:w], func=AF.Exp, scale=SCALE, bias=Y.negM[:, 0:1]),
                      reads=[pl, Y.negM], writes=[pt])
                kb.op(kb.pe, lambda pt=pt, mt=mt, h=h, w=w: nc.tensor.matmul(Y.ps_o[:, 0:w], M.vm[:, mt * 512 + h * 128:mt * 512 + (h + 1) * 128], pt[:, 0:w],
                                                                      start=(mt == 0), stop=(mt == 1)), reads=[M.vm, pt], writes=[Y.ps_o], flag=False)
                kb.op(kb.pe, lambda pt=pt, mt=mt, w=w: nc.tensor.matmul(Y.ps_r[:, 0:w], S.onesb[:], pt[:, 0:w], start=(mt == 0), stop=(mt == 1)),
                      reads=[S.onesb, pt], writes=[Y.ps_r], flag=True)
            kb.op(kb.dve, lambda w=w: nc.vector.reciprocal(Y.rinv[:, 0:w], Y.ps_r[:, 0:w]), reads=[Y.ps_r], writes=[Y.rinv])
            os_ = Y.ostg[qc % 2]
            kb.op(kb.dve, lambda os_=os_, w=w: nc.vector.tensor_tensor(os_[:, 0:w], Y.ps_o[:, 0:w], Y.rinv[:, 0:w], ALU.mult), reads=[Y.ps_o, Y.rinv], writes=[os_])
            kb.dma(kb.sp, oT_d, oT_d.t[NH + h, :, t0:t0 + w], os_, os_[:, 0:w], add=True)


def emit_wout(kb, S, og, wo_d, xT, wslots, wq, pss):
    nc = kb.nc
    w_v = wo_d.t.rearrange("(k p) c -> p k c", p=128)
    for mq in range(4):
        ws = wslots[wq["i"] % len(wslots)]; wq["i"] += 1
        wv = ws.t[:, 0:NCH * 512].rearrange("p (k c) -> p k c", c=512)
        kb.dma(kb.pool, ws, wv[:, :, :], wo_d, w_v[:, :, mq * 512:(mq + 1) * 512])
        for mi in range(4):
            m = mq * 4 + mi
            pa = pss[m % 2]
            for k in range(NCH):
                kb.op(kb.pe, lambda k=k, mi=mi, pa=pa, wv=wv: nc.tensor.matmul(pa[:], wv[:, k, mi * 128:(mi + 1) * 128], og[:, k, :],
                                                                        start=(k == 0), stop=(k == NCH - 1)),
                      reads=[ws, og], writes=[pa], flag=(k == NCH - 1))
            kb.opw(kb.dve, lambda m=m, pa=pa: nc.vector.tensor_tensor(xT[:, m, :], xT[:, m, :], pa[:], ALU.add), reads=[pa, xT], writes=[xT])


def emit_proj_tm_simple(kb, S, hT, w_d, c0, out_d, g, wslots, wq, ps_list, stg):
    nc = kb.nc
    w_v = w_d.t.rearrange("(k p) c -> p k c", p=128)
    ws = wslots[wq["i"] % len(wslots)]; wq["i"] += 1
    wv = ws.t[:, 0:NCH * 512].rearrange("p (k c) -> p k c", c=512)
    kb.dma(kb.pool, ws, wv[:, :, :], w_d, w_v[:, :, c0:c0 + 512])
    for tt in range(4):
        ps = ps_list[tt % 2]
        for k in range(NCH):
            kb.op(kb.pe, lambda k=k, ps=ps, tt=tt, wv=wv: nc.tensor.matmul(ps[:], hT[:, k, tt * 128:(tt + 1) * 128], wv[:, k, :], start=(k == 0), stop=(k == NCH - 1)),
                  reads=[ws, hT], writes=[ps], flag=(k == NCH - 1))
        ob = stg["ob"][tt % 2]
        kb.op(kb.act, lambda ps=ps, ob=ob: nc.scalar.copy(ob[:], ps[:]), reads=[ps], writes=[ob])
        tok0 = g * T + tt * 128
        kb.dma(kb.sp, out_d, out_d.t[tok0:tok0 + 128, :], ob, ob[:], add=True)


def alloc_attn_state(kb, S, Y, st, mode):
    Y.Kg = kb.sb("Kg", [128, SEQ], BF16, st)
    Y.Vg = kb.sb("Vg", [128, SEQ], BF16, st)
    Y.qall = kb.sb("qall", [128, (TPC // 128) * 384], BF16, st)
    Y.qm = kb.sb("qm", [128, TPC], BF16, st)
    Y.pT = [kb.sb("pT%d" % i, [128, 384], BF16, st) for i in range(2)]
    Y.ostg = [kb.sb("ostg%d" % i, [128, 384], BF16, st) for i in range(2)]
    Y.rinv = kb.sb("rinv", [128, 384], F32, st)
    Y.sq = [kb.sb("ysq%d" % i, [128, 512], BF16, st) for i in range(2)]
    Y.nrm = kb.sb("nrm", [128, 32], F32, st)
    Y.kmx = kb.sb("kmx", [128, 1], F32, st); Y.qmx = kb.sb("qmx", [128, 1], F32, st); Y.negM = kb.sb("negM", [128, 1], F32, st)
    Y.cm = kb.sb("ycm", [128, 128], F32, st)
    Y.ps_l = [kb.ps("ps_l%d" % i, [128, 512], F32, st) for i in range(2)]
    Y.ps_o = kb.ps("ps_o", [128, 512], F32, st)
    Y.ps_r = kb.ps("ps_r", [128, 512], F32, st)
    Y.ps_n = [kb.ps("ps_n%d" % i, [128, 512], F32, st) for i in range(2)]
    if mode == "dsa":
        Y.masks = [kb.sb("ymask%d" % i, [128, SEQ], BF16, st) for i in range(2)]


def alloc_mem_state(kb, S, M, st):
    M.xld = kb.sb("mxld", [128, D], F32, st)
    M.memT = kb.sb("memT", [128, NCH, 256], F32, st)
    M.sq = kb.sb("msq", [128, NCH, 256], BF16, st)
    M.hm = kb.sb("mhm", [128, NCH, 256], BF16, st)
    M.rstd = kb.sb("mrstd", [128, 256], F32, st)
    M.w = kb.sb("mw", [128, NCH * 512], BF16, st)
    M.ps_a = kb.ps("mps_a", [128, 512], F32, st)
    M.ps_b = kb.ps("mps_b", [128, 512], F32, st)


def load_group_kv(kb, Y, kT_d, v_d, gq):
    kb.dma(kb.sp, Y.Kg, Y.Kg[:], kT_d, kT_d.t[gq, :, :])
    kb.dma(kb.sp, Y.Vg, Y.Vg.t[:, :].rearrange("p (e d) -> p e d", d=128), v_d,
           v_d.t[:, gq * 128:(gq + 1) * 128].rearrange("(e p) d -> p e d", p=128))


def load_group_q(kb, Y, qT_d, gq):
    for h in range(3):
        kb.dma(kb.sp, Y.qall, Y.qall.t[:, :].rearrange("p (q h t) -> p q h t", h=3, t=128)[:, :, h, :], qT_d,
               qT_d.t[3 * gq + h, :, :].rearrange("p (q t) -> p q t", t=128), add=(h > 0))


def small_f32(kb, X, names, st):
    for n in names:
        setattr(X, n, kb.sb("x_" + n, [128, 128], F32, st))


def build_b(nqt=TPC // 128, ng=NG, stages=(1, 2, 3)):
    kb = KB()
    nc = kb.nc
    S = Shared()
    setup_common(kb, S)
    S.epsc = kb.sb("epsc", [128, 1], F32)
    kb.op(kb.dve, lambda: nc.vector.memset(S.epsc[:], EPS), writes=[S.epsc])
    x1_d = kb.dram("x1T", [128, NCH, TPC], F32, kind="ExternalInput")
    pos_d = kb.dram("pos", [TPC], I32, kind="ExternalInput")
    qT_d = kb.dram("qT", [NH, 128, TPC], BF16, kind="ExternalInput")
    qmT_d = kb.dram("qmT", [NMH, 128, TPC], BF16, kind="ExternalInput")
    qi_d = kb.dram("qi", [TPC, IH * IDIM], BF16, kind="ExternalInput")
    wi_d = kb.dram("wi", [TPC, IH], F32, kind="ExternalInput")
    kT_d = kb.dram("kT_all", [NKV, 128, SEQ], BF16, kind="ExternalInput")
    v_d = kb.dram("v_all", [SEQ, NKV * HD], BF16, kind="ExternalInput")
    kiT_d = kb.dram("kiT_all", [IDIM, SEQ], BF16, kind="ExternalInput")
    mem_d = kb.dram("mem", [256, D], F32, kind="ExternalInput")
    wmem_d = kb.dram("w_mem", [D, 1024], F32, kind="ExternalInput")
    wo_d = kb.dram("w_out", [D, D], F32, kind="ExternalInput")
    gu2_d = kb.dram("w_gu2", [D, 2 * DFF], F32, kind="ExternalInput")
    dn2_d = kb.dram("w_dn2", [DFF, D], F32, kind="ExternalInput")
    gu1_d = kb.dram("w_gu1", [D, 2 * DFF], F32, kind="ExternalInput")
    dn1_d = kb.dram("w_dn1", [DFF, D], F32, kind="ExternalInput")
    wkv_d = kb.dram("w_kvsh", [D, 1024], F32, kind="ExternalInput")
    wb_d = kb.dram("w_bin", [D, 2048], F32, kind="ExternalInput")
    qpos_d = kb.dram("c_qpos", [TPC], F32, kind="ExternalInput")
    iota_d = kb.dram("c_iota", [128, 1], F32, kind="ExternalInput")
    E_d = kb.dram("c_E", [128, 8], F32, kind="ExternalInput")
    g_mem = load_gain(kb, "g_mem")
    g_ffn2 = load_gain(kb, "g_ffn2")
    g_kv = load_gain(kb, "g_kv")
    g_ffn1 = load_gain(kb, "g_ffn1")
    g_att = load_gain(kb, "g_att")
    mask_d = kb.dram("mask_scr", [TPC // 128, 128, SEQ], BF16, kind="Internal")
    oT_d = kb.dram("oT_scr", [NH + NMH, 128, TPC], BF16, kind="Internal")
    x4_d = kb.dram("o_x4T", [128, NCH, TPC], F32, kind="ExternalOutput")
    q2_d = kb.dram("o_q2T", [NH, 128, TPC], BF16, kind="ExternalOutput")
    qm2_d = kb.dram("o_qm2T", [NMH, 128, TPC], BF16, kind="ExternalOutput")
    ksh_d = kb.dram("o_kshT", [NKV, 128, TPC], BF16, kind="ExternalOutput")
    vsh_d = kb.dram("o_vsh", [TPC, NKV * HD], BF16, kind="ExternalOutput")
    dbg_d = kb.dram("o_dbg", [NH + NMH, 128, TPC], BF16, kind="ExternalOutput")

    qposB = kb.sb("qposB", [128, TPC], F32)
    kb.dma(kb.sp, qposB, qposB[:], qpos_d, qpos_d.t.partition_broadcast(128))
    iota = kb.sb("iota", [128, 1], F32)
    kb.dma(kb.sp, iota, iota[:], iota_d, iota_d[:])
    M = Shared()
    M.kmT = kb.sb("kmT", [128, NMH * 256], BF16)
    M.vm = kb.sb("vm", [128, 2 * 512], BF16)
    with contextlib.ExitStack() as st:
        alloc_mem_state(kb, S, M, st)
        emit_mem_kv(kb, S, M, mem_d, g_mem, wmem_d)
        kb.barrier()
    if 1 in stages:
        with contextlib.ExitStack() as st:
            X = Shared()
            X.qposB = qposB; X.iota = iota; X.identf = S.ident
            X.kiT = kb.sb("x_kiT", [64, SEQ], BF16, st)
            kb.dma(kb.sp, X.kiT, X.kiT[:], kiT_d, kiT_d[:])
            X.iscT = kb.sb("x_iscT", [128, SEQ], F32, st)
            X.ind = kb.sb("x_ind", [128, SEQ], BF16, st)
            X.qrows = kb.sb("x_qrows", [128, 16 * IDIM], BF16, st)
            X.qgT = kb.sb("x_qgT", [64, 16 * 128], BF16, st)
            X.wg = kb.sb("x_wg", [128, 128], BF16, st)
            X.wT = kb.sb("x_wT", [128, TPC // 8], F32, st)
            kb.dma(kb.sp, X.wT, X.wT[:], wi_d, wi_d.t.rearrange("(g t) h -> (t h) g", t=8), allow_slow_non_contiguous=True)
            X.E = kb.sb("x_E", [128, 8], F32, st)
            kb.dma(kb.sp, X.E, X.E[:], E_d, E_d[:])
            X.R = [kb.sb("x_R%d" % i, [128, 512], BF16, st) for i in range(2)]
            small_f32(kb, X, ["pmax", "pmin", "hi", "lo", "mid", "c1", "sel", "d1", "cm", "cmb", "diag", "onesf"], st)
            kb.op(kb.dve, lambda: nc.vector.memset(X.onesf[:], 1.0), writes=[X.onesf])
            X.col = kb.sb("x_col", [128, 1], F32, st)
            X.ps_dots = [kb.ps("ps_dots%d" % i, [128, 512], F32, st) for i in range(2)]
            X.ps_isc = [kb.ps("ps_isc%d" % i, [128, 512], F32, st) for i in range(2)]
            X.ps_cnt = kb.ps("ps_cnt", [128, 512], F32, st)
            X.ps_sm = kb.ps("ps_sm", [128, 128], F32, st)
            X.ps_qt = kb.ps("ps_qt", [128, 2048], BF16, st)
            for qt in range(nqt):
                emit_indexer_qtile(kb, S, X, qt, qt // 4, qi_d, mask_d)
            kb.barrier()
    if 2 in stages:
        with contextlib.ExitStack() as st:
            Y = Shared()
            Y.qposB = qposB; Y.iota = iota
            alloc_attn_state(kb, S, Y, st, "dsa")
            for gq in range(NKV):
                load_group_kv(kb, Y, kT_d, v_d, gq)
                load_group_q(kb, Y, qT_d, gq)
                emit_stab(kb, S, Y, Y.qall, (TPC // 128) * 384)
                for qt in range(nqt):
                    E = uniform_extent(qt // 4) * 4
                    Y.mask = Y.masks[qt % 2]
                    kb.dma(kb.sp, Y.mask, Y.mask[:, 0:E * 128], mask_d, mask_d.t[qt, :, 0:E * 128])
                    emit_attn_qtile(kb, S, Y, qt, qt // 4, gq, "dsa", oT_d, 3 * gq)
            emit_mem_attn(kb, S, M, Y, qmT_d, oT_d, nqt)
            kb.barrier()
    if 3 in stages:
        with contextlib.ExitStack() as st:
            S.es_save = kb.es
            kb.es = st
            alloc_ffn_state_nb(kb, S)
            og = kb.sb("og", [128, NCH, T], BF16)
            stg = dict(ob=[kb.sb("ob%d" % i, [128, T], BF16) for i in range(2)],
                       a=[kb.sb("a%d" % i, [128, T], F32) for i in range(2)],
                       t1=[kb.sb("t1%d" % i, [128, T], F32) for i in range(2)],
                       t2=[kb.sb("t2%d" % i, [128, T], F32) for i in range(2)])
            kb.es = S.es_save
            for g in range(ng):
                kb.dma(kb.sp, S.xT, S.xT[:, :, :], x1_d, x1_d.t[:, :, g * T:(g + 1) * T])
                kb.dma(kb.sp, og, og[:, :, :], oT_d, oT_d.t[:, :, g * T:(g + 1) * T].rearrange("h p t -> p h t"))
                emit_rope_tables(kb, S, pos_d, g)
                emit_wout(kb, S, og, wo_d, S.xT, S.wslots, S.wq, S.ps_g)
                emit_norm(kb, S, S.xT, g_ffn2, S.hT, S.act, S.ps_ss, S.rstd)
                emit_ffn(kb, S, S.xT, S.hT, S.act, S.wslots, S.wq, gu2_d, dn2_d, S.ps_g, S.ps_u, S.ps_y, S.sil)
                emit_norm(kb, S, S.xT, g_kv, S.hT, S.act, S.ps_ss, S.rstd)
                emit_proj_fm(kb, S, S.hT, wkv_d, 0, NKV, True, ksh_d, g, S.wslots, S.wq, (S.ps_g, S.ps_u), stg)
                emit_proj_tm_simple(kb, S, S.hT, wkv_d, 512, vsh_d, g, S.wslots, S.wq, S.ps_y, stg)
                emit_norm(kb, S, S.xT, g_ffn1, S.hT, S.act, S.ps_ss, S.rstd)
                emit_ffn(kb, S, S.xT, S.hT, S.act, S.wslots, S.wq, gu1_d, dn1_d, S.ps_g, S.ps_u, S.ps_y, S.sil)
                emit_store_xT(kb, S.xT, x4_d, g)
                emit_norm(kb, S, S.xT, g_att, S.hT, S.act, S.ps_ss, S.rstd)
                emit_proj_fm(kb, S, S.hT, wb_d, 0, NH, True, q2_d, g, S.wslots, S.wq, (S.ps_g, S.ps_u), stg)
                emit_proj_fm(kb, S, S.hT, wb_d, 1536, NMH, False, qm2_d, g, S.wslots, S.wq, (S.ps_g, S.ps_u), stg)
            kb.barrier()
    dstg = kb.sb("dstg", [128, TPC], BF16)
    for h in range(NH + NMH):
        kb.dma(kb.sp, dstg, dstg[:], oT_d, oT_d.t[h, :, :])
        kb.dma(kb.sp, dbg_d, dbg_d.t[h, :, :], dstg, dstg[:], add=True)
    nc = kb.finish()
    return nc, kb


NBLK = SEQ // 256


def emit_moba_gate(kb, S, Y, qt):
    nc = kb.nc
    q3 = Y.qall[:, qt * 384:(qt + 1) * 384]
    for h in range(3):
        kb.op(kb.pe, lambda h=h: nc.tensor.matmul(Y.ps_gate[:, h * 64:(h + 1) * 64], q3[:, h * 128:(h + 1) * 128], Y.kmT[:, 0:64], start=True, stop=True),
              reads=[Y.qall, Y.kmT], writes=[Y.ps_gate], flag=(h == 2))
    g3 = Y.gate.t[:, :].rearrange("p (h n) -> p h n", n=64)
    kb.op(kb.dve, lambda: nc.vector.tensor_tensor(g3, Y.ps_gate.t[:, 0:192].rearrange("p (h n) -> p h n", n=64),
                                                   Y.validB[:, qt * 64:(qt + 1) * 64].unsqueeze(1).to_broadcast([128, 3, 64]), ALU.add),
          reads=[Y.ps_gate, Y.validB], writes=[Y.gate])
    for h in range(3):
        kb.op(kb.dve, lambda h=h: nc.vector.max(out=Y.mx8[:], in_=Y.gate[:, h * 64:(h + 1) * 64]), reads=[Y.gate], writes=[Y.mx8])
        kb.opw(kb.dve, lambda h=h: nc.vector.tensor_scalar(Y.sel[:, h * 64:(h + 1) * 64], Y.gate[:, h * 64:(h + 1) * 64], Y.mx8[:, 2:3], None, ALU.is_ge),
               reads=[Y.gate, Y.mx8], writes=[Y.sel])
    s3 = Y.sel.t[:, :].rearrange("p (h n) -> p h n", n=64)
    kb.op(kb.dve, lambda: nc.vector.tensor_tensor(s3, s3, Y.valid01[:, qt * 64:(qt + 1) * 64].unsqueeze(1).to_broadcast([128, 3, 64]), ALU.mult),
          reads=[Y.sel, Y.valid01], writes=[Y.sel])
    kb.op(kb.dve, lambda: nc.vector.tensor_tensor(s3, s3, Y.own01[:, qt * 64:(qt + 1) * 64].unsqueeze(1).to_broadcast([128, 3, 64]), ALU.add),
          reads=[Y.sel, Y.own01], writes=[Y.sel])
    kb.op(kb.dve, lambda: nc.vector.tensor_scalar(Y.biasb[:], Y.sel[:], -1.0, 1.0e5, ALU.add, ALU.mult), reads=[Y.sel], writes=[Y.biasb])
    for h in range(3):
        kb.op(kb.pe, lambda h=h: nc.tensor.transpose(Y.ps_bt[0:64, h * 128:(h + 1) * 128], Y.biasb[:, h * 64:(h + 1) * 64], S.identb[:]),
              reads=[Y.biasb, S.identb], writes=[Y.ps_bt], flag=(h == 2))
    kb.op(kb.act, lambda: nc.scalar.copy(Y.biasT[0:64, :], Y.ps_bt[0:64, 0:384]), reads=[Y.ps_bt], writes=[Y.biasT])


def emit_out_tm(kb, S, yT, out_d, g, otile, ps_list):
    nc = kb.nc
    n = 0
    for tt in range(4):
        ot = otile[tt % 2]
        for cq in range(4):
            pt = ps_list[n % 2]; n += 1
            for ci in range(4):
                c = cq * 4 + ci
                kb.op(kb.pe, lambda pt=pt, ci=ci, c=c, tt=tt: nc.tensor.transpose(pt[:, ci * 128:(ci + 1) * 128], yT[:, c, tt * 128:(tt + 1) * 128], S.ident[:]),
                      reads=[yT, S.ident], writes=[pt], flag=(ci == 3))
            if n % 2:
                kb.opw(kb.act, lambda pt=pt, cq=cq, ot=ot: nc.scalar.copy(ot[:, cq * 512:(cq + 1) * 512], pt[:]), reads=[pt], writes=[ot])
            else:
                kb.opw(kb.dve, lambda pt=pt, cq=cq, ot=ot: nc.vector.tensor_copy(ot[:, cq * 512:(cq + 1) * 512], pt[:]), reads=[pt], writes=[ot])
        tok0 = g * T + tt * 128
        kb.dma(kb.sp, out_d, out_d.t[tok0:tok0 + 128, :], ot, ot[:], add=True)


def emit_norm_f32(kb, S, xT, gain, yT, sq, ps_ss, rstd):
    nc = kb.nc
    half = NCH // 2
    for hh in range(2):
        kb.opw(kb.act, lambda hh=hh: nc.scalar.activation(out=sq[:, hh * half:(hh + 1) * half, :], in_=xT[:, hh * half:(hh + 1) * half, :], func=AF.Square),
               reads=[xT], writes=[sq])
    for c in range(NCH):
        kb.op(kb.pe, lambda c=c: nc.tensor.matmul(ps_ss[:], S.onesb[:], sq[:, c, :], start=(c == 0), stop=(c == NCH - 1)),
              reads=[S.onesb, sq], writes=[ps_ss], flag=(c == NCH - 1))
    kb.op(kb.act, lambda: nc.scalar.activation(out=rstd[:], in_=ps_ss[:], func=AF.Sqrt, scale=1.0 / D, bias=S.epsc[:, 0:1]),
          reads=[ps_ss, S.epsc], writes=[rstd])
    kb.op(kb.dve, lambda: nc.vector.reciprocal(rstd[:], rstd[:]), reads=[rstd], writes=[rstd])
    for c in range(NCH):
        kb.opw(kb.dve, lambda c=c: nc.vector.scalar_tensor_tensor(yT[:, c, :], xT[:, c, :], gain[:, c:c + 1], rstd[:], ALU.mult, ALU.mult),
               reads=[xT, gain, rstd], writes=[yT])


def build_c(nqt=TPC // 128, ng=NG, stages=(2, 3)):
    kb = KB()
    nc = kb.nc
    S = Shared()
    setup_common(kb, S)
    S.epsc = kb.sb("epsc", [128, 1], F32)
    kb.op(kb.dve, lambda: nc.vector.memset(S.epsc[:], EPS), writes=[S.epsc])
    x4_d = kb.dram("x4T", [128, NCH, TPC], F32, kind="ExternalInput")
    qT_d = kb.dram("qT", [NH, 128, TPC], BF16, kind="ExternalInput")
    qmT_d = kb.dram("qmT", [NMH, 128, TPC], BF16, kind="ExternalInput")
    kT_d = kb.dram("kT_all", [NKV, 128, SEQ], BF16, kind="ExternalInput")
    v_d = kb.dram("v_all", [SEQ, NKV * HD], BF16, kind="ExternalInput")
    mem_d = kb.dram("mem", [256, D], F32, kind="ExternalInput")
    wmem_d = kb.dram("w_mem", [D, 1024], F32, kind="ExternalInput")
    wo_d = kb.dram("w_out", [D, D], F32, kind="ExternalInput")
    gu2_d = kb.dram("w_gu2", [D, 2 * DFF], F32, kind="ExternalInput")
    dn2_d = kb.dram("w_dn2", [DFF, D], F32, kind="ExternalInput")
    qpos_d = kb.dram("c_qpos", [TPC], F32, kind="ExternalInput")
    iota_d = kb.dram("c_iota", [128, 1], F32, kind="ExternalInput")
    valid_d = kb.dram("c_validB", [(TPC // 128) * 64], F32, kind="ExternalInput")
    valid01_d = kb.dram("c_valid01", [(TPC // 128) * 64], F32, kind="ExternalInput")
    own_d = kb.dram("c_own01", [(TPC // 128) * 64], F32, kind="ExternalInput")
    g_mem = load_gain(kb, "g_mem")
    g_ffn2 = load_gain(kb, "g_ffn2")
    g_fin = load_gain(kb, "g_fin")
    oT_d = kb.dram("oT_scr", [NH + NMH, 128, TPC], BF16, kind="Internal")
    out_d = kb.dram("o_out", [TPC, D], F32, kind="ExternalOutput")
    dbg_d = kb.dram("o_dbg", [NH + NMH, 128, TPC], BF16, kind="ExternalOutput")
    qposB = kb.sb("qposB", [128, TPC], F32)
    kb.dma(kb.sp, qposB, qposB[:], qpos_d, qpos_d.t.partition_broadcast(128))
    iota = kb.sb("iota", [128, 1], F32)
    kb.dma(kb.sp, iota, iota[:], iota_d, iota_d[:])
    M = Shared()
    M.kmT = kb.sb("kmT", [128, NMH * 256], BF16)
    M.vm = kb.sb("vm", [128, 2 * 512], BF16)
    with contextlib.ExitStack() as st:
        alloc_mem_state(kb, S, M, st)
        emit_mem_kv(kb, S, M, mem_d, g_mem, wmem_d)
        kb.barrier()
    if 2 in stages:
        with contextlib.ExitStack() as st:
            Y = Shared()
            Y.qposB = qposB; Y.iota = iota
            alloc_attn_state(kb, S, Y, st, "moba")
            nq64 = (TPC // 128) * 64
            for nm, dd in (("validB", valid_d), ("valid01", valid01_d), ("own01", own_d)):
                b = kb.sb("y_" + nm, [128, nq64], F32, st)
                kb.dma(kb.sp, b, b[:], dd, dd.t.partition_broadcast(128))
                setattr(Y, nm, b)
            Y.kmf = kb.sb("y_kmf", [128, 64], F32, st)
            Y.kmT = kb.sb("y_kmT", [128, 64], BF16, st)
            Y.gate = kb.sb("y_gate", [128, 192], F32, st)
            Y.sel = kb.sb("y_sel", [128, 192], F32, st)
            Y.mx8 = kb.sb("y_mx8", [128, 8], F32, st)
            Y.biasb = kb.sb("y_biasb", [128, 192], BF16, st)
            Y.biasT = kb.sb("y_biasT", [64, 384], BF16, st)
            Y.En = kb.sb("y_En", [64, 64 * 128], BF16, st)
            kb.op(kb.dve, lambda: nc.vector.tensor_copy(Y.En.t[:, :].rearrange("p (n m) -> p n m", m=128),
                                                         S.ident[0:64, 0:64].unsqueeze(2).to_broadcast([64, 64, 128])),
                  reads=[S.ident], writes=[Y.En])
            Y.ps_gate = kb.ps("ps_gate", [128, 256], F32, st)
            Y.ps_bt = kb.ps("ps_bt", [128, 512], BF16, st)
            for gq in range(NKV):
                load_group_kv(kb, Y, kT_d, v_d, gq)
                load_group_q(kb, Y, qT_d, gq)
                kb.op(kb.dve, lambda: nc.vector.tensor_reduce(Y.kmf[:], Y.Kg.t[:, :].rearrange("p (n s) -> p n s", s=256), AX.X, ALU.add),
                      reads=[Y.Kg], writes=[Y.kmf])
                kb.op(kb.act, lambda: nc.scalar.mul(Y.kmT[:], Y.kmf[:], 1.0 / 256.0), reads=[Y.kmf], writes=[Y.kmT])
                emit_stab(kb, S, Y, Y.qall, (TPC // 128) * 384)
                for qt in range(nqt):
                    emit_moba_gate(kb, S, Y, qt)
                    emit_attn_qtile(kb, S, Y, qt, qt // 4, gq, "moba", oT_d, 3 * gq)
            emit_mem_attn(kb, S, M, Y, qmT_d, oT_d, nqt)
            kb.barrier()
    if 3 in stages:
        with contextlib.ExitStack() as st:
            S.es_save = kb.es
            kb.es = st
            alloc_ffn_state_nb(kb, S)
            og = kb.sb("og", [128, NCH, T], BF16)
            yT = S.xT
            otile = [kb.sb("otile%d" % i, [128, D], F32) for i in range(2)]
            kb.es = S.es_save
            for g in range(ng):
                kb.dma(kb.sp, S.xT, S.xT[:, :, :], x4_d, x4_d.t[:, :, g * T:(g + 1) * T])
                kb.dma(kb.sp, og, og[:, :, :], oT_d, oT_d.t[:, :, g * T:(g + 1) * T].rearrange("h p t -> p h t"))
                emit_wout(kb, S, og, wo_d, S.xT, S.wslots, S.wq, S.ps_g)
                emit_norm(kb, S, S.xT, g_ffn2, S.hT, S.act, S.ps_ss, S.rstd)
                emit_ffn(kb, S, S.xT, S.hT, S.act, S.wslots, S.wq, gu2_d, dn2_d, S.ps_g, S.ps_u, S.ps_y, S.sil)
                emit_norm_f32(kb, S, S.xT, g_fin, yT, S.act, S.ps_ss, S.rstd)
                emit_out_tm(kb, S, yT, out_d, g, otile, S.ps_y)
            kb.barrier()
    dstg = kb.sb("dstg", [128, TPC], BF16)
    for h in range(NH + NMH):
        kb.dma(kb.sp, dstg, dstg[:], oT_d, oT_d.t[h, :, :])
        kb.dma(kb.sp, dbg_d, dbg_d.t[h, :, :], dstg, dstg[:], add=True)
    nc = kb.finish()
    return nc, kb


def _c(a):
    return np.ascontiguousarray(a)


def run_a(inp, ng=NG, cores=NCORE):
    nc, kb = build_a(ng)
    cs = consts()
    maps = []
    for c in range(cores):
        tok = core_tokens(c)
        m = dict(cs)
        m["x"] = _c(inp["x"][0][tok])
        m["pos"] = _c(inp["positions"][0][tok]).astype(np.int32)
        m["w_gu"] = _c(inp["ffn1_w_gate_up"][0])
        m["w_dn"] = _c(inp["ffn1_w_down"][0])
        m["w_in"] = _c(inp["a_w_in"][0])
        m["g_ffn"] = _c(inp["ffn1_norm"][0])
        m["g_att"] = _c(inp["attn_norm"][0])
        m["g_idxk"] = _c(inp["idx_k_norm"][0])
        maps.append(m)
    res = run_bass_kernel_spmd(nc, maps, core_ids=list(range(cores)))
    return res.results


def gather_tokens_last(parts, key):
    shp = parts[0][key].shape
    out = np.zeros(shp[:-1] + (SEQ,), dtype=parts[0][key].dtype)
    for c in range(NCORE):
        out[..., core_tokens(c)] = parts[c][key]
    return out


def gather_tokens_first(parts, key):
    shp = parts[0][key].shape
    out = np.zeros((SEQ,) + shp[1:], dtype=parts[0][key].dtype)
    for c in range(NCORE):
        out[core_tokens(c)] = parts[c][key]
    return out


def b_consts(c):
    E = np.zeros((128, 8), np.float32)
    for t in range(8):
        E[t * 16:(t + 1) * 16, t] = 1.0
    return dict(c_qpos=core_tokens(c).astype(np.float32), c_iota=np.arange(128, dtype=np.float32).reshape(128, 1), c_E=E)


def run_b(inp, ra, cores=NCORE, **kw):
    nc, kb = build_b(**kw)
    cs = consts()
    kT_all = gather_tokens_last(ra, "o_kT")
    kiT_all = gather_tokens_last(ra, "o_kiT")
    v_all = gather_tokens_first(ra, "o_v")
    maps = []
    for c in range(cores):
        tok = core_tokens(c)
        m = dict(cs)
        m.update(b_consts(c))
        m["x1T"] = ra[c]["o_x1T"]; m["qT"] = ra[c]["o_qT"]; m["qmT"] = ra[c]["o_qmT"]; m["qi"] = ra[c]["o_qi"]; m["wi"] = ra[c]["o_wi"]
        m["pos"] = _c(inp["positions"][0][tok]).astype(np.int32)
        m["kT_all"] = kT_all; m["v_all"] = v_all; m["kiT_all"] = kiT_all
        m["mem"] = _c(inp["mem"][0]); m["w_mem"] = _c(inp["w_mem_kv"][0]); m["w_out"] = _c(inp["w_out"][0])
        m["w_gu2"] = _c(inp["ffn2_w_gate_up"][0]); m["w_dn2"] = _c(inp["ffn2_w_down"][0])
        m["w_gu1"] = _c(inp["ffn1_w_gate_up"][1]); m["w_dn1"] = _c(inp["ffn1_w_down"][1])
        m["w_kvsh"] = _c(inp["w_kv_shared"]); m["w_bin"] = _c(inp["b_w_in"][0])
        m["g_mem"] = _c(inp["mem_norm"][0]); m["g_ffn2"] = _c(inp["ffn2_norm"][0]); m["g_kv"] = _c(inp["kv_norm"])
        m["g_ffn1"] = _c(inp["ffn1_norm"][1]); m["g_att"] = _c(inp["attn_norm"][1])
        maps.append(m)
    res = run_bass_kernel_spmd(nc, maps, core_ids=list(range(cores)))
    return res.results


def c_consts(c):
    nq = TPC // 128
    validB = np.zeros((nq, 64), np.float32); valid01 = np.zeros((nq, 64), np.float32); own01 = np.zeros((nq, 64), np.float32)
    tok = core_tokens(c)
    for qt in range(nq):
        cur = int(tok[qt * 128]) // 256
        n = np.arange(64)
        valid01[qt] = (n < cur)
        validB[qt] = np.where(n < cur, 0.0, -1.0e30)
        own01[qt] = (n == cur)
    return dict(c_qpos=tok.astype(np.float32), c_iota=np.arange(128, dtype=np.float32).reshape(128, 1),
                c_validB=validB.reshape(-1), c_valid01=valid01.reshape(-1), c_own01=own01.reshape(-1))


def run_c(inp, rb, cores=NCORE, **kw):
    nc, kb = build_c(**kw)
    cs = consts()
    kT_all = gather_tokens_last(rb, "o_kshT")
    v_all = gather_tokens_first(rb, "o_vsh")
    maps = []
    for c in range(cores):
        m = dict(cs)
        m.update(c_consts(c))
        m["x4T"] = rb[c]["o_x4T"]; m["qT"] = rb[c]["o_q2T"]; m["qmT"] = rb[c]["o_qm2T"]
        m["kT_all"] = kT_all; m["v_all"] = v_all
        m["mem"] = _c(inp["mem"][0]); m["w_mem"] = _c(inp["w_mem_kv"][1]); m["w_out"] = _c(inp["w_out"][1])
        m["w_gu2"] = _c(inp["ffn2_w_gate_up"][1]); m["w_dn2"] = _c(inp["ffn2_w_down"][1])
        m["g_mem"] = _c(inp["mem_norm"][1]); m["g_ffn2"] = _c(inp["ffn2_norm"][1]); m["g_fin"] = _c(inp["final_norm"])
        maps.append(m)
    res = run_bass_kernel_spmd(nc, maps, core_ids=list(range(cores)))
    return res.results


def kernel(**inp):
    inp = {k: np.asarray(v) for k, v in inp.items()}
    ra = run_a(inp)
    rb = run_b(inp, ra)
    rc = run_c(inp, rb)
    out = np.zeros((1, SEQ, D), np.float32)
    for c in range(NCORE):
        out[0, core_tokens(c)] = rc[c]["o_out"]
    return out
```
